# Optimizing a Trainium2 kernel written in Bass

```python
import math
import jax
import jax.numpy as jnp
from jax import lax
import numpy as np

D_MODEL = 1024
BATCH = 8
SEQ = 4096
DEPTH = 1

MIX_WIDTH = D_MODEL
HYENA_WIDTH = MIX_WIDTH // 2
HYENA_ORDER = 2
HYENA_GROUPS = 8
SHORT_CONV_WIDTH = 3
FILTER_EMB_DIM = 33
FILTER_BANDS = (FILTER_EMB_DIM - 1) // 2
FILTER_HIDDEN = 64
DECAY_TARGET = 1e-2
FAST_DECAY_PCT = 0.3
SLOW_DECAY_PCT = 1.5
ATTN_WIDTH = MIX_WIDTH - HYENA_WIDTH
ATTN_HEADS = 4
ATTN_HEAD_DIM = ATTN_WIDTH // (2 * ATTN_HEADS)
Q_BLOCK = 128
HYENA_COLS = (HYENA_ORDER + 1) * HYENA_WIDTH
IN_COLS = HYENA_COLS + 3 * ATTN_WIDTH
N_EXPERTS = 16
EC_CAPACITY_FACTOR = 2
EXPERT_FF = 2816
NORM_EPS = 1e-6

kernel_name = 'hymba_hyena_diffattn_ec_block'


def _rmsnorm(x, g):
    xf = x.astype(jnp.float32)
    y = xf * lax.rsqrt(jnp.mean(xf * xf, axis=-1, keepdims=True) + NORM_EPS)
    return (y * g.astype(jnp.float32)).astype(x.dtype)


def _alibi_slopes(n):
    return jnp.asarray([2.0 ** (-8.0 * (i + 1) / n) for i in range(n)], dtype=jnp.float32)


def _hyena_filters(L, w1, b1, w2, b2, w3, freq):
    f32 = jnp.float32
    t = jnp.linspace(0.0, 1.0, L, dtype=f32)[:, None]
    w = 2.0 * math.pi * jnp.arange(L, dtype=f32)[:, None] / L
    bands = jnp.linspace(1e-4, FILTER_BANDS - 1, FILTER_BANDS, dtype=f32)[None, :]
    z = jnp.concatenate([t, jnp.cos(bands * w), -jnp.sin(bands * w)], axis=-1)
    fr = freq.astype(f32)
    hid = jnp.sin(fr * (z @ w1.astype(f32) + b1.astype(f32)))
    hid = jnp.sin(fr * (hid @ w2.astype(f32) + b2.astype(f32)))
    h = (hid @ w3.astype(f32)).reshape(L, 2, HYENA_ORDER, HYENA_WIDTH)
    min_decay = math.log(DECAY_TARGET) / SLOW_DECAY_PCT
    max_decay = math.log(DECAY_TARGET) / FAST_DECAY_PCT
    deltas = jnp.abs(jnp.linspace(min_decay, max_decay, HYENA_WIDTH, dtype=f32))
    h = h * jnp.exp(-t[:, :, None, None] * deltas)
    fwd, bwd = h[:, 0], h[:, 1]
    k = jnp.concatenate([fwd, jnp.zeros((1, HYENA_ORDER, HYENA_WIDTH), f32), bwd[:0:-1]], axis=0)
    return k / jnp.sum(jnp.abs(k), axis=0, keepdims=True)


def _fft_conv(u, k_freq, L):
    uf = jnp.fft.rfft(u, n=2 * L, axis=1)
    return jnp.fft.irfft(uf * k_freq[None], n=2 * L, axis=1)[:, :L]


def _hyena_mixer(proj, conv_w, conv_b, w1, b1, w2, b2, w3, freq, skip, norm_g):
    B, L, _ = proj.shape
    pad = SHORT_CONV_WIDTH // 2
    up = jnp.pad(proj, ((0, 0), (pad, pad), (0, 0)))
    u = conv_b
    for j in range(SHORT_CONV_WIDTH):
        u = u + up[:, j:j + L] * conv_w[j]
    x1, x2, v = jnp.split(u, HYENA_ORDER + 1, axis=-1)
    k_freq = jnp.fft.rfft(_hyena_filters(L, w1, b1, w2, b2, w3, freq), axis=0)
    skip = skip.astype(jnp.float32)
    z = v.astype(jnp.float32)
    for n, gate in enumerate((x1, x2)):
        z = gate.astype(jnp.float32) * (_fft_conv(z, k_freq[:, n], L) + skip[n] * z)
    z = z.reshape(B, L, HYENA_GROUPS, HYENA_WIDTH // HYENA_GROUPS)
    z = _rmsnorm(z, norm_g.reshape(HYENA_GROUPS, HYENA_WIDTH // HYENA_GROUPS))
    return z.reshape(B, L, HYENA_WIDTH).astype(proj.dtype)


def _diff_attention(q, k, v, lam, lam_init, subln_g):
    B, L, _ = q.shape
    H, Dh = ATTN_HEADS, ATTN_HEAD_DIM
    q = q.reshape(B, L, H, 2, Dh)
    k = k.reshape(B, L, H, 2, Dh)
    v = v.reshape(B, L, H, 2 * Dh)
    scale = Dh ** -0.5
    slopes = _alibi_slopes(H)
    nb = L // Q_BLOCK
    qb = q.reshape(B, nb, Q_BLOCK, H, 2, Dh).transpose(1, 0, 2, 3, 4, 5)
    starts = jnp.arange(nb, dtype=jnp.int32) * Q_BLOCK
    kpos = jnp.arange(L, dtype=jnp.float32)

    def block(args):
        qblk, s0 = args
        qpos = (s0 + jnp.arange(Q_BLOCK, dtype=jnp.int32)).astype(jnp.float32)
        dist = jnp.abs(qpos[:, None] - kpos[None, :])
        s = jnp.einsum('bqhcd,bkhcd->bhcqk', qblk, k).astype(jnp.float32) * scale
        s = s - slopes[None, :, None, None, None] * dist
        p = jax.nn.softmax(s, axis=-1)
        a = p[:, :, 0] - lam * p[:, :, 1]
        return jnp.einsum('bhqk,bkhe->bqhe', a.astype(v.dtype), v)

    o = lax.map(block, (qb, starts))
    o = o.transpose(1, 0, 2, 3, 4).reshape(B, L, H, 2 * Dh)
    o = _rmsnorm(o, subln_g) * (1.0 - lam_init)
    return o.reshape(B, L, H * 2 * Dh).astype(q.dtype)


def _expert_choice_ffn(h, w_router, w_gate, w_up, w_down):
    B, L, _ = h.shape
    cap = EC_CAPACITY_FACTOR * L // N_EXPERTS
    logits = jnp.einsum('bsd,de->bse', h, w_router).astype(jnp.float32)
    aff = jax.nn.softmax(logits, axis=-1)
    g, idx = lax.top_k(aff.transpose(2, 0, 1), cap)
    bi = jnp.arange(B, dtype=jnp.int32)[None, :, None]
    xin = h[bi, idx]

    def expert(args):
        xe, wg, wu, wd = args
        a = jnp.einsum('bcd,df->bcf', xe, wg)
        u = jnp.einsum('bcd,df->bcf', xe, wu)
        return jnp.einsum('bcf,fd->bcd', jax.nn.silu(a) * u, wd)

    out = lax.map(expert, (xin, w_gate, w_up, w_down))
    out = out * g[..., None].astype(out.dtype)
    return jnp.zeros_like(h).at[bi, idx].add(out)


def setup_inputs(seed: int = 0) -> dict:
    key = jax.random.key(seed)
    ks = jax.random.split(key, 25)
    f32 = jnp.float32

    def nrm(k, shape, scale):
        return jax.random.normal(k, shape, f32) * scale

    D, E, F = D_MODEL, N_EXPERTS, EXPERT_FF
    return {
        'x': nrm(ks[0], (BATCH, SEQ, D), 1.0),
        'attn_norm_g': 1.0 + nrm(ks[1], (DEPTH, D), 0.02),
        'w_in': nrm(ks[2], (DEPTH, D, IN_COLS), D ** -0.5),
        'conv_w': nrm(ks[3], (DEPTH, SHORT_CONV_WIDTH, HYENA_COLS), SHORT_CONV_WIDTH ** -0.5),
        'conv_b': nrm(ks[4], (DEPTH, HYENA_COLS), 0.02),
        'filt_w1': nrm(ks[5], (DEPTH, FILTER_EMB_DIM, FILTER_HIDDEN), FILTER_EMB_DIM ** -0.5),
        'filt_b1': nrm(ks[6], (DEPTH, FILTER_HIDDEN), 0.1),
        'filt_w2': nrm(ks[7], (DEPTH, FILTER_HIDDEN, FILTER_HIDDEN), FILTER_HIDDEN ** -0.5),
        'filt_b2': nrm(ks[8], (DEPTH, FILTER_HIDDEN), 0.1),
        'filt_w3': nrm(ks[9], (DEPTH, FILTER_HIDDEN, 2 * HYENA_ORDER * HYENA_WIDTH), FILTER_HIDDEN ** -0.5),
        'filt_freq': 1.0 + nrm(ks[10], (DEPTH, FILTER_HIDDEN), 0.1),
        'hyena_skip': nrm(ks[11], (DEPTH, HYENA_ORDER, HYENA_WIDTH), 1.0),
        'hyena_norm_g': 1.0 + nrm(ks[12], (DEPTH, HYENA_WIDTH), 0.02),
        'lambda_q1': nrm(ks[13], (DEPTH, ATTN_HEAD_DIM), 0.1),
        'lambda_k1': nrm(ks[14], (DEPTH, ATTN_HEAD_DIM), 0.1),
        'lambda_q2': nrm(ks[15], (DEPTH, ATTN_HEAD_DIM), 0.1),
        'lambda_k2': nrm(ks[16], (DEPTH, ATTN_HEAD_DIM), 0.1),
        'subln_g': 1.0 + nrm(ks[17], (DEPTH, 2 * ATTN_HEAD_DIM), 0.02),
        'w_out': nrm(ks[18], (DEPTH, MIX_WIDTH, D), MIX_WIDTH ** -0.5),
        'ffn_norm_g': 1.0 + nrm(ks[19], (DEPTH, D), 0.02),
        'w_router': nrm(ks[20], (DEPTH, D, E), D ** -0.5),
        'w_gate': nrm(ks[21], (DEPTH, E, D, F), D ** -0.5),
        'w_up': nrm(ks[22], (DEPTH, E, D, F), D ** -0.5),
        'w_down': nrm(ks[23], (DEPTH, E, F, D), F ** -0.5),
        'final_norm_g': 1.0 + nrm(ks[24], (D,), 0.02),
    }


def reference(x, attn_norm_g, w_in, conv_w, conv_b, filt_w1, filt_b1, filt_w2, filt_b2,
              filt_w3, filt_freq, hyena_skip, hyena_norm_g, lambda_q1, lambda_k1,
              lambda_q2, lambda_k2, subln_g, w_out, ffn_norm_g, w_router, w_gate, w_up,
              w_down, final_norm_g):
    f32 = jnp.float32
    for l in range(DEPTH):
        hn = _rmsnorm(x, attn_norm_g[l])
        proj = jnp.einsum('bsd,dn->bsn', hn, w_in[l])
        hy = _hyena_mixer(proj[..., :HYENA_COLS], conv_w[l], conv_b[l], filt_w1[l], filt_b1[l],
                          filt_w2[l], filt_b2[l], filt_w3[l], filt_freq[l], hyena_skip[l],
                          hyena_norm_g[l])
        q, k, v = jnp.split(proj[..., HYENA_COLS:], 3, axis=-1)
        lam_init = 0.8 - 0.6 * math.exp(-0.3 * l)
        lam = (jnp.exp(jnp.sum(lambda_q1[l].astype(f32) * lambda_k1[l].astype(f32)))
               - jnp.exp(jnp.sum(lambda_q2[l].astype(f32) * lambda_k2[l].astype(f32)))
               + lam_init)
        at = _diff_attention(q, k, v, lam, lam_init, subln_g[l])
        mix = jnp.concatenate([hy, at], axis=-1)
        x = x + jnp.einsum('bsm,md->bsd', mix, w_out[l])
        x = x + _expert_choice_ffn(_rmsnorm(x, ffn_norm_g[l]), w_router[l], w_gate[l],
                                   w_up[l], w_down[l])
    return _rmsnorm(x, final_norm_g)
```

```python
import math
import os
from contextlib import ExitStack

import numpy as np
import concourse.bass as bass
import concourse.mybir as mybir
from concourse.bass_utils import run_bass_kernel_spmd

F32 = mybir.dt.float32
BF16 = mybir.dt.bfloat16
I32 = mybir.dt.int32
ALU = mybir.AluOpType
AF = mybir.ActivationFunctionType
AX = mybir.AxisListType

L = 4096
D = 1024
NT = L // 128
HW = 512
NE = 16
CAP = 512
FF = 2816
NFC = FF // 128
EPS = 1e-6
NFFT = 2 * L

SAME_ENG_SYNC = True


class Instr:
    __slots__ = ("eng", "fn", "deps", "dma", "signal", "sigidx", "lane", "val")

    def __init__(self, eng, fn, dma):
        self.eng = eng
        self.fn = fn
        self.dma = dma
        self.deps = {}
        self.signal = False
        self.sigidx = -1
        self.lane = None
        self.val = 0


class Sched:
    ENG = ("pe", "act", "dve", "pool", "sp")
    NL = 4
    ND = 12

    def __init__(self, nc, stack):
        self.nc = nc
        self.esem = {}
        for e in ("pe", "act", "dve", "pool"):
            self.esem[e] = [stack.enter_context(nc.semaphore(f"es_{e}{i}")) for i in range(self.NL)]
        self.dsem = {}
        for q in ("sp", "act", "pool"):
            self.dsem[q] = [stack.enter_context(nc.semaphore(f"ds_{q}{i}")) for i in range(self.ND)]
        self.phase_sem = stack.enter_context(nc.semaphore("phase"))
        self.phase = 0
        self.sigcount = {e: 0 for e in self.ENG}
        self.dmacount = {q: 0 for q in ("sp", "act", "pool")}
        self.pending = {e: [] for e in self.ENG}
        self.state = {}
        self.known = {e: {s: -1 for s in self.ENG} for e in self.ENG}
        self.known_dma = {e: {} for e in self.ENG}
        self.bar_tile = stack.enter_context(nc.sbuf_tensor("bar_tile", [128, 8], F32))
        self.n_instr = 0

    def op(self, eng, fn, reads=(), writes=(), dma=False):
        ins = Instr(eng, fn, dma)
        st = self.state
        for k in reads:
            s = st.get(k)
            if s is not None and s[0] is not None:
                ins.deps[id(s[0])] = (s[0], "raw")
        for k in writes:
            s = st.get(k)
            if s is not None:
                if s[0] is not None:
                    ins.deps[id(s[0])] = (s[0], "raw")
                for r in s[1].values():
                    if id(r) not in ins.deps:
                        ins.deps[id(r)] = (r, "war")
                for r in s[2]:
                    if id(r) not in ins.deps:
                        ins.deps[id(r)] = (r, "war")
        for k in reads:
            s = st.get(k)
            if s is None:
                s = [None, {}, []]
                st[k] = s
            if dma:
                s[2].append(ins)
            else:
                s[1][eng] = ins
        for k in writes:
            st[k] = [ins, {}, []]
        self.pending[eng].append(ins)
        self.n_instr += 1
        return ins

    @staticmethod
    def _needs_wait(ins, d, kind):
        if d.dma or ins.dma:
            return True
        if d.eng != ins.eng:
            return True
        if ins.eng == "pe":
            return False
        if kind == "war":
            return False
        return SAME_ENG_SYNC

    def flush(self):
        nc = self.nc
        pend = self.pending
        for e in self.ENG:
            for ins in pend[e]:
                for (d, kind) in ins.deps.values():
                    if not d.dma and self._needs_wait(ins, d, kind):
                        d.signal = True
        last_sig = {}
        for e in ("pe", "act", "dve", "pool"):
            comp = [i for i in pend[e] if not i.dma]
            if comp:
                comp[-1].signal = True
        for e in self.ENG:
            for ins in pend[e]:
                if ins.dma:
                    j = self.dmacount[e]
                    self.dmacount[e] += 1
                    ins.lane = j % self.ND
                    ins.val = 16 * (j // self.ND + 1)
                elif ins.signal:
                    ins.sigidx = self.sigcount[e]
                    self.sigcount[e] += 1
        for e in ("pe", "act", "dve", "pool"):
            last_sig[e] = self.sigcount[e] - 1
        phase = self.phase
        sched = self

        def wait_sig(eobj, me, src, sigidx):
            if sched.known[me][src] >= sigidx:
                return
            eobj.wait_ge(sched.esem[src][sigidx % sched.NL], sigidx // sched.NL + 1)
            sched.known[me][src] = sigidx

        def wait_dma(eobj, me, q, lane, val):
            kd = sched.known_dma[me]
            if kd.get((q, lane), 0) >= val:
                return
            eobj.wait_ge(sched.dsem[q][lane], val)
            kd[(q, lane)] = val

        def run(me, eobj):
            if phase > 0:
                eobj.wait_ge(sched.phase_sem, phase)
            for ins in pend[me]:
                for (d, kind) in ins.deps.values():
                    if not sched._needs_wait(ins, d, kind):
                        continue
                    if d.dma:
                        wait_dma(eobj, me, d.eng, d.lane, d.val)
                    else:
                        wait_sig(eobj, me, d.eng, d.sigidx)
                if ins.dma:
                    if ins.val > 16:
                        wait_dma(eobj, me, me, ins.lane, ins.val - 16)
                    r = ins.fn(eobj)
                    r.then_inc(sched.dsem[me][ins.lane], 16)
                else:
                    r = ins.fn(eobj)
                    if ins.signal:
                        r.then_inc(sched.esem[me][ins.sigidx % sched.NL], 1)
            if me == "pool":
                for src in ("pe", "act", "dve"):
                    if last_sig[src] >= 0:
                        wait_sig(eobj, me, src, last_sig[src])
                if last_sig["pool"] >= 0 and SAME_ENG_SYNC:
                    wait_sig(eobj, me, "pool", last_sig["pool"])
                for q in ("sp", "act", "pool"):
                    n = sched.dmacount[q]
                    for lane in range(sched.ND):
                        cnt = (n - lane + sched.ND - 1) // sched.ND if n > lane else 0
                        if cnt > 0:
                            wait_dma(eobj, me, q, lane, 16 * cnt)
                eobj.memset(sched.bar_tile[:], 0.0).then_inc(sched.phase_sem, 1)

        with nc.Block() as block:
            @block.tensor
            def _(e):
                run("pe", e)

            @block.scalar
            def _(e):
                run("act", e)

            @block.vector
            def _(e):
                run("dve", e)

            @block.gpsimd
            def _(e):
                run("pool", e)

            @block.sync
            def _(e):
                run("sp", e)

        self.phase += 1
        self.pending = {e: [] for e in self.ENG}
        self.state = {}

    def final_wait(self):
        nc = self.nc
        phase = self.phase
        sched = self
        with nc.Block() as block:
            @block.tensor
            def _(e):
                e.wait_ge(sched.phase_sem, phase)

            @block.scalar
            def _(e):
                e.wait_ge(sched.phase_sem, phase)

            @block.vector
            def _(e):
                e.wait_ge(sched.phase_sem, phase)

            @block.gpsimd
            def _(e):
                e.wait_ge(sched.phase_sem, phase)

            @block.sync
            def _(e):
                e.wait_ge(sched.phase_sem, phase)


def bcast_rows(ap, nparts):
    return ap.partition_broadcast(nparts)


class Ctx:
    pass


C_last = {}


def build_program(debug=None):
    nc = bass.Bass("TRN2", target_bir_lowering=False)
    C = Ctx()
    C.nc = nc
    C.debug = debug

    SHAPES = {
        "x": [L, D], "attn_norm_g": [1, D], "w_in": [D, 3072], "conv_w": [3, 1536], "conv_b": [1, 1536],
        "filt_w1": [33, 64], "filt_b1": [64, 1], "filt_w2": [64, 64], "filt_b2": [64, 1],
        "filt_w3": [64, 2048], "filt_freq": [64, 1], "hyena_skip": [2, 512], "hyena_norm_g": [1, 512],
        "lambda_q1": [1, 64], "lambda_k1": [1, 64], "lambda_q2": [1, 64], "lambda_k2": [1, 64],
        "subln_g": [128, 1], "w_out": [D, D], "ffn_norm_g": [1, D], "w_router": [D, NE],
        "w_gate": [NE, D, FF], "w_up": [NE, D, FF], "w_down": [NE, FF, D], "final_norm_g": [1, D],
    }

    class LazyIn(dict):
        def __missing__(self, name):
            ap = nc.dram_tensor(name, list(SHAPES[name]), F32, kind="ExternalInput").ap()
            self[name] = ap
            return ap

    I = LazyIn()
    C.SHAPES = SHAPES
    C.I = I
    C.out = nc.dram_tensor("out", [L, D], F32, kind="ExternalOutput").ap()

    def dscratch(name, shape, dt):
        if debug:
            return nc.dram_tensor(name, list(shape), dt, kind="ExternalOutput").ap()
        return nc.dram_tensor(name, list(shape), dt).ap()

    C.dscratch = dscratch
    with ExitStack() as stack:
        S = Sched(nc, stack)
        C.S = S
        C.stack = stack
        C.ps = [stack.enter_context(nc.psum_tensor(f"ps{i}", [128, 512], F32)) for i in range(8)]
        C.ident = stack.enter_context(nc.sbuf_tensor("ident", [128, 128], F32))
        C.identb = stack.enter_context(nc.sbuf_tensor("identb", [128, 128], BF16))
        C.ones_f = stack.enter_context(nc.sbuf_tensor("ones_f", [128, 128], F32))
        C.ones_b = stack.enter_context(nc.sbuf_tensor("ones_b", [128, 128], BF16))
        S.op("pool", lambda e: e.memset(C.ones_f[:], 1.0), writes=["ones_f"])
        S.op("pool", lambda e: e.memset(C.ones_b[:], 1.0), writes=["ones_b"])
        C.tmp_id = stack.enter_context(nc.sbuf_tensor("tmp_id", [128, 128], F32))
        S.op("pool", lambda e: e.iota(C.tmp_id[:], [[1, 128]], base=0, channel_multiplier=-1,
                                      allow_small_or_imprecise_dtypes=True), writes=["tmp_id"])
        S.op("dve", lambda e: e.tensor_scalar(C.ident[:], C.tmp_id[:], 0.0, None, ALU.is_equal),
             reads=["tmp_id"], writes=["ident"])
        S.op("dve", lambda e: e.tensor_copy(C.identb[:], C.ident[:]), reads=["ident"], writes=["identb"])
        S.flush()

        C.MIXT = C.dscratch("MIXT", [8, 128, L], BF16)
        stages = [("proj", "phase_proj"), ("attn", "phase_attn"), ("dft", "phase_dftgen"), ("filt", "phase_filter"),
                  ("conv", "phase_conv"), ("oproj", "phase_oproj"), ("topk", "phase_topk"), ("moe", "phase_moe"),
                  ("final", "phase_final")]
        only = os.environ.get("MK_ONLY")
        for name, fname in stages:
            if only and name not in only.split(","):
                continue
            globals()[fname](C)
            if debug == name:
                break
        S.final_wait()
    C_last["I"] = I
    C_last["SHAPES"] = SHAPES
    return nc


def precast_experts(C, e0, e1, dep_keys=()):
    nc, S, I = C.nc, C.S, C.I
    if not hasattr(C, "WGB"):
        NP_ALL = 11
        C.WGB = C.dscratch("WGB", [NP_ALL, D, FF], BF16)
        C.WUB = C.dscratch("WUB", [NP_ALL, D, FF], BF16)
        C.WDB = C.dscratch("WDB", [NP_ALL, FF, D], BF16)
        C.NPRE = 0
    dep_keys = list(dep_keys)
    for ex in range(e0, e1):
        for (src, dst, nm) in ((I["w_gate"], C.WGB, "g"), (I["w_up"], C.WUB, "u")):
            sv = src[ex].rearrange("d (a f) -> (d a) f", a=2)
            dv = dst[ex].rearrange("d (a f) -> (d a) f", a=2)
            for q in range(4):
                S.op("pool", lambda e, sv=sv, dv=dv, q=q: e.dma_start(out=dv[q * 512:(q + 1) * 512, :],
                                                                     in_=sv[q * 512:(q + 1) * 512, :]),
                     reads=dep_keys, writes=[f"W{nm}B{ex}_{q}"], dma=True)
        for q in range(4):
            S.op("pool", lambda e, ex=ex, q=q: e.dma_start(out=C.WDB[ex, q * 704:(q + 1) * 704, :],
                                                           in_=I["w_down"][ex, q * 704:(q + 1) * 704, :]),
                 reads=dep_keys, writes=[f"WdB{ex}_{q}"], dma=True)
    C.NPRE = max(C.NPRE, e1)


def phase_proj(C):
    nc, S, I = C.nc, C.S, C.I
    C.U = [C.dscratch(f"U{g}", [L, HW], F32) for g in range(3)]
    C.QT = C.dscratch("QT", [4, 128, L], BF16)
    C.KT = C.dscratch("KT", [4, 128, L], BF16)
    C.VA = C.dscratch("VA", [L, HW], BF16)
    C.VH = C.dscratch("VH", [L, HW], BF16)
    with ExitStack() as st:
        def sb(name, shape, dt):
            return st.enter_context(nc.sbuf_tensor(name, shape, dt))

        hnT = sb("hnT", [128, 8, L + 2], BF16)
        g32 = sb("g32", [128, D], F32)
        xt = [sb(f"xt{i}", [128, D], F32) for i in range(2)]
        hn = [sb(f"hn{i}", [128, D], F32) for i in range(2)]
        junk = sb("junk", [128, D], F32)
        ss = [sb(f"ss{i}", [128, 1], F32) for i in range(2)]
        rstd = [sb(f"rstd{i}", [128, 1], F32) for i in range(2)]

        S.op("sp", lambda e: e.dma_start(out=g32[:], in_=bcast_rows(I["attn_norm_g"], 128)),
             writes=["g32"], dma=True)
        S.op("dve", lambda e: e.tensor_scalar(g32[:], g32[:], 32.0, None, ALU.mult),
             reads=["g32"], writes=["g32"])
        S.op("pool", lambda e: e.memset(hnT[:, :, 0:1], 0.0), writes=["hnT_pad0"])
        S.op("pool", lambda e: e.memset(hnT[:, :, L + 1:L + 2], 0.0), writes=["hnT_pad1"])
        precast_experts(C, 8, 11)

        for i in range(NT):
            b = i % 2
            S.op("sp", lambda e, i=i, b=b: e.dma_start(out=xt[b][:], in_=I["x"][i * 128:(i + 1) * 128, :]),
                 writes=[f"xt{b}"], dma=True)
            S.op("dve", lambda e, b=b: e.memset(ss[b][:], 0.0), writes=[f"ss{b}"])
            S.op("act", lambda e, b=b: e.activation(out=junk[:], in_=xt[b][:], func=AF.Square,
                                                    accum_out=ss[b][:]),
                 reads=[f"xt{b}", f"ss{b}"], writes=[f"ss{b}", "junk"])
            S.op("act", lambda e, b=b: e.activation(out=ss[b][:], in_=ss[b][:], func=AF.Sqrt,
                                                    bias=float(D * EPS), scale=1.0),
                 reads=[f"ss{b}"], writes=[f"ss{b}"])
            S.op("dve", lambda e, b=b: e.reciprocal(rstd[b][:], ss[b][:]),
                 reads=[f"ss{b}"], writes=[f"rstd{b}"])
            S.op("dve", lambda e, b=b: e.scalar_tensor_tensor(out=hn[b][:], in0=xt[b][:], scalar=rstd[b][:, 0:1],
                                                              in1=g32[:], op0=ALU.mult, op1=ALU.mult),
                 reads=[f"xt{b}", f"rstd{b}", "g32"], writes=[f"hn{b}"])
            for half in range(2):
                pb = (2 * i + half) % 2
                for kk in range(4):
                    k = half * 4 + kk
                    S.op("pe", lambda e, b=b, k=k, kk=kk, pb=pb: e.transpose(
                        out=C.ps[pb][:, kk * 128:(kk + 1) * 128], in_=hn[b][:, k * 128:(k + 1) * 128],
                        identity=C.ident[:]),
                        reads=[f"hn{b}", "ident"], writes=[f"ps{pb}"])
                eng = "act" if half == 0 else "dve"
                if eng == "act":
                    S.op("act", lambda e, i=i, half=half, pb=pb: e.copy(
                        out=hnT[:, half * 4:(half + 1) * 4, 1 + i * 128:1 + (i + 1) * 128],
                        in_=C.ps[pb][:].rearrange("p (k t) -> p k t", k=4)),
                        reads=[f"ps{pb}"], writes=[f"hnT_{i}_{half}"])
                else:
                    S.op("dve", lambda e, i=i, half=half, pb=pb: e.tensor_copy(
                        out=hnT[:, half * 4:(half + 1) * 4, 1 + i * 128:1 + (i + 1) * 128],
                        in_=C.ps[pb][:].rearrange("p (k t) -> p k t", k=4)),
                        reads=[f"ps{pb}"], writes=[f"hnT_{i}_{half}"])
        hnT_keys = [f"hnT_{i}_{h}" for i in range(NT) for h in range(2)] + ["hnT_pad0", "hnT_pad1"]

        wst = [sb(f"wst{i}", [128, 8, 512], F32) for i in range(2)]
        wbf = [sb(f"wbf{i}", [128, 8, 3, 512], BF16) for i in range(2)]
        cw = sb("cw", [128, 3, 1536], F32)
        cb = sb("cb", [128, 1536], F32)
        for j in range(3):
            S.op("sp", lambda e, j=j: e.dma_start(out=cw[:, j, :], in_=bcast_rows(I["conv_w"][j:j + 1, :], 128)),
                 writes=[f"cw{j}"], dma=True)
        S.op("sp", lambda e: e.dma_start(out=cb[:], in_=bcast_rows(I["conv_b"], 128)), writes=["cb"], dma=True)
        ost = [sb(f"ost{i}", [128, 512], F32) for i in range(3)]
        obf = [sb(f"obf{i}", [128, 512], BF16) for i in range(3)]
        oT = [sb(f"oT{i}", [128, 512], BF16) for i in range(3)]
        w_view = I["w_in"].rearrange("(k p) n -> p k n", p=128)
        psb = [2, 3, 4, 5]
        pcount = [0]
        ocount = [0]
        scale_q = 64 ** -0.5
        for g in range(6):
            wb = g % 2
            S.op("sp", lambda e, g=g, wb=wb: e.dma_start(out=wst[wb][:], in_=w_view[:, :, g * 512:(g + 1) * 512]),
                 writes=[f"wst{wb}"], dma=True)
            if g < 3:
                for j in range(3):
                    eng = "dve"
                    S.op(eng, lambda e, g=g, wb=wb, j=j: e.tensor_tensor(
                        out=wbf[wb][:, :, j, :], in0=wst[wb][:],
                        in1=cw[:, j, g * 512:(g + 1) * 512].unsqueeze(1).to_broadcast([128, 8, 512]),
                        op=ALU.mult),
                        reads=[f"wst{wb}", f"cw{j}"], writes=[f"wbf{wb}_{j}"])
                wkeys = [f"wbf{wb}_{j}" for j in range(3)]
            else:
                S.op("act", lambda e, wb=wb: e.copy(out=wbf[wb][:, :, 0, :], in_=wst[wb][:]),
                     reads=[f"wst{wb}"], writes=[f"wbf{wb}_0"])
                wkeys = [f"wbf{wb}_0"]
            if g in (0, 1, 2, 5):
                ntap = 3 if g < 3 else 1
                for i in range(NT):
                    pb = psb[pcount[0] % 4]
                    pcount[0] += 1
                    n_mm = ntap * 8
                    c = 0
                    for j in range(ntap):
                        for k in range(8):
                            off = i * 128 + j if g < 3 else i * 128 + 1
                            S.op("pe", lambda e, pb=pb, k=k, j=j, off=off, wb=wb, c=c, n_mm=n_mm: e.matmul(
                                C.ps[pb][:], lhsT=hnT[:, k, off:off + 128], rhs=wbf[wb][:, k, j, :],
                                start=(c == 0), stop=(c == n_mm - 1)),
                                reads=hnT_keys_for(i) + wkeys, writes=[f"ps{pb}"])
                            c += 1
                    ob = ocount[0] % 3
                    ocount[0] += 1
                    if g < 3:
                        S.op("dve", lambda e, pb=pb, ob=ob, g=g: e.tensor_tensor(
                            out=ost[ob][:], in0=C.ps[pb][:], in1=cb[:, g * 512:(g + 1) * 512], op=ALU.add),
                            reads=[f"ps{pb}", "cb"], writes=[f"ost{ob}"])
                        S.op("sp", lambda e, ob=ob, g=g, i=i: e.dma_start(
                            out=C.U[g][i * 128:(i + 1) * 128, :], in_=ost[ob][:]),
                            reads=[f"ost{ob}"], writes=[f"U{g}_{i}"], dma=True)
                        if g == 2:
                            S.op("act", lambda e, ob=ob: e.copy(out=obf[ob][:], in_=ost[ob][:]),
                                 reads=[f"ost{ob}"], writes=[f"obf{ob}"])
                            S.op("sp", lambda e, ob=ob, i=i: e.dma_start(
                                out=C.VH[i * 128:(i + 1) * 128, :], in_=obf[ob][:]),
                                reads=[f"obf{ob}"], writes=[f"VH_{i}"], dma=True)
                    else:
                        S.op("act", lambda e, pb=pb, ob=ob: e.copy(out=obf[ob][:], in_=C.ps[pb][:]),
                             reads=[f"ps{pb}"], writes=[f"obf{ob}"])
                        S.op("sp", lambda e, ob=ob, i=i: e.dma_start(
                            out=C.VA[i * 128:(i + 1) * 128, :], in_=obf[ob][:]),
                            reads=[f"obf{ob}"], writes=[f"VA_{i}"], dma=True)
            else:
                dst = C.QT if g == 3 else C.KT
                for h in range(4):
                    for tb in range(8):
                        pb = psb[pcount[0] % 4]
                        pcount[0] += 1
                        for k in range(8):
                            S.op("pe", lambda e, pb=pb, k=k, h=h, tb=tb, wb=wb: e.matmul(
                                C.ps[pb][:], lhsT=wbf[wb][:, k, 0, h * 128:(h + 1) * 128],
                                rhs=hnT[:, k, 1 + tb * 512:1 + (tb + 1) * 512],
                                start=(k == 0), stop=(k == 7)),
                                reads=hnT_keys_blk(tb) + wkeys, writes=[f"ps{pb}"])
                        ob = ocount[0] % 3
                        ocount[0] += 1
                        if g == 3:
                            S.op("act", lambda e, pb=pb, ob=ob: e.mul(out=oT[ob][:], in_=C.ps[pb][:], mul=scale_q),
                                 reads=[f"ps{pb}"], writes=[f"oT{ob}"])
                        else:
                            S.op("dve", lambda e, pb=pb, ob=ob: e.tensor_copy(out=oT[ob][:], in_=C.ps[pb][:]),
                                 reads=[f"ps{pb}"], writes=[f"oT{ob}"])
                        S.op("sp", lambda e, ob=ob, h=h, tb=tb, dst=dst: e.dma_start(
                            out=dst[h, :, tb * 512:(tb + 1) * 512], in_=oT[ob][:]),
                            reads=[f"oT{ob}"], writes=[f"{'QT' if dst is C.QT else 'KT'}_{h}_{tb}"], dma=True)
        S.flush()


def hnT_keys_for(i):
    ks = [f"hnT_{i}_0", f"hnT_{i}_1"]
    if i > 0:
        ks += [f"hnT_{i - 1}_0", f"hnT_{i - 1}_1"]
    else:
        ks += ["hnT_pad0"]
    if i < NT - 1:
        ks += [f"hnT_{i + 1}_0", f"hnT_{i + 1}_1"]
    else:
        ks += ["hnT_pad1"]
    return ks


def hnT_keys_blk(tb):
    ks = []
    for i in range(tb * 4, tb * 4 + 4):
        ks += [f"hnT_{i}_0", f"hnT_{i}_1"]
    return ks


C_last = {}


def kernel(**inputs):
    debug = os.environ.get("MK_DEBUG")
    x = np.asarray(inputs["x"], dtype=np.float32)
    nc = build_program(debug)
    common = {}
    for name in C_last["I"].keys():
        if name == "x":
            continue
        common[name] = np.ascontiguousarray(
            np.asarray(inputs[name], dtype=np.float32).reshape(C_last["SHAPES"][name]))
    in_maps = []
    for c in range(8):
        m = dict(common)
        m["x"] = np.ascontiguousarray(x[c])
        in_maps.append(m)
    res = run_bass_kernel_spmd(nc, in_maps, core_ids=list(range(8)))
    if debug:
        return res
    out = np.stack([np.asarray(r["out"], dtype=np.float32) for r in res.results], axis=0)
    return out


def phase_attn(C):
    nc, S, I = C.nc, C.S, C.I
    if not hasattr(C, "QT"):
        C.QT = C.dscratch("QT", [4, 128, L], BF16)
        C.KT = C.dscratch("KT", [4, 128, L], BF16)
        C.VA = C.dscratch("VA", [L, HW], BF16)
    XTAB = C.dscratch("XTAB", [4, 3, 4, L], BF16)
    KR = 68
    with ExitStack() as st0:
        def sb0(name, shape, dt):
            return st0.enter_context(nc.sbuf_tensor(name, shape, dt))
        PJ = L // 128
        pos = sb0("apos", [128, PJ], F32)
        posi = sb0("aposi", [128, PJ], I32)
        plo = sb0("aplo", [128, PJ], F32)
        phi = sb0("aphi", [128, PJ], F32)
        NR = 2 + 4 * 4
        rowt = sb0("arows", [128, NR, PJ], BF16)
        S.op("pool", lambda e: e.iota(pos[:], [[1, PJ]], base=0, channel_multiplier=PJ,
                                      allow_small_or_imprecise_dtypes=True), writes=["apos"])
        S.op("pool", lambda e: e.iota(posi[:], [[1, PJ]], base=0, channel_multiplier=PJ), writes=["aposi"])
        S.op("dve", lambda e: e.tensor_scalar(posi[:], posi[:], 127, None, ALU.bitwise_and), reads=["aposi"], writes=["aposi"])
        S.op("dve", lambda e: e.tensor_copy(plo[:], posi[:]), reads=["aposi"], writes=["aplo"])
        S.op("dve", lambda e: e.tensor_tensor(out=phi[:], in0=pos[:], in1=plo[:], op=ALU.subtract),
             reads=["apos", "aplo"], writes=["aphi"])
        S.op("pool", lambda e: e.memset(rowt[:, 0, :], 1.0), writes=["arow_one"])
        S.op("pool", lambda e: e.memset(rowt[:, 1, :], -1.0), writes=["arow_none"])
        ridx = {"one": 0, "none": 1}
        for h in range(4):
            slope = 2.0 ** (-8.0 * (h + 1) / 4)
            for k_, (nm, src, sk, mul) in enumerate((("a", phi, "aphi", slope), ("b", plo, "aplo", slope),
                                                     ("na", phi, "aphi", -slope), ("nb", plo, "aplo", -slope))):
                ri = 2 + 4 * h + k_
                ridx[(h, nm)] = ri
                S.op("dve", lambda e, ri=ri, src=src, mul=mul: e.tensor_scalar(rowt[:, ri, :], src[:], float(mul), None, ALU.mult),
                     reads=[sk], writes=[f"arow{ri}"])
            layout = (("one", "one", "na", "nb"), ("a", "b", "one", "one"), ("na", "nb", "none", "none"))
            for v, names in enumerate(layout):
                for rr, nm in enumerate(names):
                    ri = ridx[nm] if nm in ("one", "none") else ridx[(h, nm)]
                    key = f"arow_{nm}" if nm in ("one", "none") else f"arow{ri}"
                    S.op("sp", lambda e, h=h, v=v, rr=rr, ri=ri: e.dma_start(
                        out=XTAB[h, v, rr, :].rearrange("(p j) -> p j", j=PJ), in_=rowt[:, ri, :]),
                        reads=[key], writes=[f"XTAB{h}_{v}_{rr}"], dma=True)
        S.flush()
    with ExitStack() as st:
        def sb(name, shape, dt):
            return st.enter_context(nc.sbuf_tensor(name, shape, dt))

        qa = [[sb(f"qa{i}_{c}", [KR, L], BF16) for c in range(2)] for i in range(2)]
        ka = [[[sb(f"ka{i}_{c}_{v}", [KR, L], BF16) for v in range(2)] for c in range(2)] for i in range(2)]
        vall = sb("vall", [128, NT, HW], BF16)
        D0 = sb("D0", [128, 512], F32)
        Dr = [sb(f"Dr{r}", [128, 512], F32) for r in range(4)]
        lamv = [sb(f"lamv{i}", [128, 64], F32) for i in range(4)]
        lt = [sb(f"lt{i}", [128, 1], F32) for i in range(4)]
        neg_lam = sb("neg_lam", [128, 1], F32)
        gsc = sb("gsc", [128, 1], F32)
        stt = [sb(f"stt{i}", [128, 512], F32) for i in range(4)]
        ptt = [sb(f"ptt{i}", [128, 512], BF16) for i in range(4)]
        r0 = sb("r0", [128, 512], F32)
        t0 = sb("t0", [128, 512], F32)
        r1 = sb("r1", [128, 512], F32)
        t1 = sb("t1", [128, 512], F32)
        oo = sb("oo", [128, 512], F32)
        sq = sb("sq", [128, 512], F32)
        rs = sb("rs", [128, 512], F32)
        res = [sb(f"res{i}", [128, 512], BF16) for i in range(2)]
        S.op("sp", lambda e: e.dma_start(out=vall[:], in_=C.VA.rearrange("(i p) n -> p i n", p=128)),
             writes=["vall"], dma=True)
        S.op("pool", lambda e: e.iota(D0[:], [[1, 512]], base=0, channel_multiplier=-1,
                                      allow_small_or_imprecise_dtypes=True), writes=["D0"])
        for r in range(4):
            S.op("dve", lambda e, r=r: e.tensor_scalar(Dr[r][:], D0[:], -1.0, float(128 * r), ALU.mult, ALU.add),
                 reads=["D0"], writes=[f"Dr{r}"])
            S.op("dve", lambda e, r=r: e.tensor_scalar(Dr[r][:], Dr[r][:], 0.0, 2.0, ALU.max, ALU.mult),
                 reads=[f"Dr{r}"], writes=[f"Dr{r}"])
        for i, nm in enumerate(["lambda_q1", "lambda_k1", "lambda_q2", "lambda_k2"]):
            S.op("sp", lambda e, i=i, nm=nm: e.dma_start(out=lamv[i][:], in_=bcast_rows(I[nm], 128)),
                 writes=[f"lamv{i}"], dma=True)
        S.op("sp", lambda e: e.dma_start(out=gsc[:], in_=I["subln_g"]), writes=["gsc"], dma=True)
        lam_init = 0.8 - 0.6 * math.exp(0.0)
        S.op("dve", lambda e: e.tensor_scalar(gsc[:], gsc[:], float(1.0 - lam_init), None, ALU.mult),
             reads=["gsc"], writes=["gsc"])
        for pi in range(2):
            S.op("dve", lambda e, pi=pi: e.tensor_tensor(out=lamv[2 * pi][:], in0=lamv[2 * pi][:],
                                                         in1=lamv[2 * pi + 1][:], op=ALU.mult),
                 reads=[f"lamv{2 * pi}", f"lamv{2 * pi + 1}"], writes=[f"lamv{2 * pi}"])
            S.op("dve", lambda e, pi=pi: e.reduce_sum(out=lt[pi][:], in_=lamv[2 * pi][:], axis=AX.X),
                 reads=[f"lamv{2 * pi}"], writes=[f"lt{pi}"])
            S.op("act", lambda e, pi=pi: e.activation(out=lt[2 + pi][:], in_=lt[pi][:], func=AF.Exp),
                 reads=[f"lt{pi}"], writes=[f"lt{2 + pi}"])
        S.op("dve", lambda e: e.tensor_tensor(out=neg_lam[:], in0=lt[3][:], in1=lt[2][:], op=ALU.subtract),
             reads=["lt2", "lt3"], writes=["neg_lam"])
        S.op("dve", lambda e: e.tensor_scalar(neg_lam[:], neg_lam[:], float(-lam_init), None, ALU.add),
             reads=["neg_lam"], writes=["neg_lam"])

        units = [(h, b, a) for h in range(4) for b in range(8) for a in range(NT)]
        LAG = 1
        U = len(units)

        def load_head(h):
            hb = h % 2
            for c in range(2):
                S.op("sp", lambda e, c=c: e.dma_start(out=qa[hb][c][0:64, :], in_=C.QT[h, c * 64:(c + 1) * 64, :]),
                     writes=[f"qa{hb}_{c}"], dma=True)
                S.op("sp", lambda e, c=c: e.dma_start(out=qa[hb][c][64:68, :], in_=XTAB[h, 0]),
                     writes=[f"qa{hb}_{c}x"], dma=True)
                for v in range(2):
                    S.op("sp", lambda e, c=c, v=v: e.dma_start(out=ka[hb][c][v][0:64, :], in_=C.KT[h, c * 64:(c + 1) * 64, :]),
                         writes=[f"ka{hb}_{c}_{v}"], dma=True)
                    S.op("sp", lambda e, c=c, v=v: e.dma_start(out=ka[hb][c][v][64:68, :], in_=XTAB[h, 1 + v]),
                         writes=[f"ka{hb}_{c}_{v}x"], dma=True)

        def stage_a(u):
            h, b, a = units[u]
            slope = 2.0 ** (-8.0 * (h + 1) / 4)
            hb = h % 2
            if (h, b, a) == (0, 0, 0):
                load_head(0)
            if (b, a) == (2, 0) and h + 1 < 4:
                load_head(h + 1)
            v = 1 if a > 4 * b + 3 else 0
            diag = (4 * b <= a <= 4 * b + 3)
            for c in range(2):
                sbk = (2 * u + c) % 4
                psn = 4 + sbk
                S.op("pe", lambda e, c=c, psn=psn: e.matmul(
                    C.ps[psn][:], lhsT=ka[hb][c][v][:, a * 128:(a + 1) * 128],
                    rhs=qa[hb][c][:, b * 512:(b + 1) * 512], start=True, stop=True),
                    reads=[f"qa{hb}_{c}", f"qa{hb}_{c}x", f"ka{hb}_{c}_{v}", f"ka{hb}_{c}_{v}x"], writes=[f"ps{psn}"])
            for c in range(2):
                sbk = (2 * u + c) % 4
                psn = 4 + sbk
                if diag:
                    r = a - 4 * b
                    S.op("dve", lambda e, r=r, sbk=sbk, psn=psn: e.scalar_tensor_tensor(
                        out=stt[sbk][:], in0=Dr[r][:], scalar=float(-slope), in1=C.ps[psn][:], op0=ALU.mult, op1=ALU.add),
                        reads=[f"Dr{r}", f"ps{psn}"], writes=[f"stt{sbk}"])
                    S.op("act", lambda e, sbk=sbk: e.activation(out=ptt[sbk][:], in_=stt[sbk][:], func=AF.Exp),
                         reads=[f"stt{sbk}"], writes=[f"ptt{sbk}"])
                else:
                    S.op("act", lambda e, sbk=sbk, psn=psn: e.activation(out=ptt[sbk][:], in_=C.ps[psn][:], func=AF.Exp),
                         reads=[f"ps{psn}"], writes=[f"ptt{sbk}"])

        def stage_b(u):
            h, b, a = units[u]
            for c in range(2):
                sbk = (2 * u + c) % 4
                S.op("pe", lambda e, c=c, sbk=sbk: e.matmul(
                    C.ps[2 * c][:], lhsT=vall[:, a, h * 128:(h + 1) * 128], rhs=ptt[sbk][:],
                    start=(a == 0), stop=(a == NT - 1)),
                    reads=["vall", f"ptt{sbk}"], writes=[f"ps{2 * c}"])
            for c in range(2):
                sbk = (2 * u + c) % 4
                S.op("pe", lambda e, c=c, sbk=sbk: e.matmul(
                    C.ps[2 * c + 1][:], lhsT=C.ones_b[:], rhs=ptt[sbk][:],
                    start=(a == 0), stop=(a == NT - 1)),
                    reads=["ones_b", f"ptt{sbk}"], writes=[f"ps{2 * c + 1}"])
            if a != NT - 1:
                return
            S.op("dve", lambda e: e.reciprocal(r0[:], C.ps[1][:]), reads=["ps1"], writes=["r0"])
            S.op("dve", lambda e: e.tensor_tensor(out=t0[:], in0=C.ps[0][:], in1=r0[:], op=ALU.mult),
                 reads=["ps0", "r0"], writes=["t0"])
            S.op("dve", lambda e: e.reciprocal(r1[:], C.ps[3][:]), reads=["ps3"], writes=["r1"])
            S.op("dve", lambda e: e.tensor_tensor(out=t1[:], in0=C.ps[2][:], in1=r1[:], op=ALU.mult),
                 reads=["ps2", "r1"], writes=["t1"])
            S.op("dve", lambda e: e.scalar_tensor_tensor(out=oo[:], in0=t1[:], scalar=neg_lam[:, 0:1], in1=t0[:],
                                                         op0=ALU.mult, op1=ALU.add),
                 reads=["t1", "t0", "neg_lam"], writes=["oo"])
            S.op("dve", lambda e: e.tensor_tensor(out=sq[:], in0=oo[:], in1=oo[:], op=ALU.mult),
                 reads=["oo"], writes=["sq"])
            S.op("pe", lambda e: e.matmul(C.ps[1][:], lhsT=C.ones_f[:], rhs=sq[:], start=True, stop=True),
                 reads=["ones_f", "sq"], writes=["ps1"])
            S.op("act", lambda e: e.activation(out=rs[:], in_=C.ps[1][:], func=AF.Sqrt,
                                               bias=float(EPS), scale=1.0 / 128.0),
                 reads=["ps1"], writes=["rs"])
            S.op("dve", lambda e: e.reciprocal(rs[:], rs[:]), reads=["rs"], writes=["rs"])
            rb = (h * 8 + b) % 2
            S.op("dve", lambda e: e.scalar_tensor_tensor(out=res[rb][:], in0=oo[:], scalar=gsc[:, 0:1],
                                                         in1=rs[:], op0=ALU.mult, op1=ALU.mult),
                 reads=["oo", "gsc", "rs"], writes=[f"res{rb}"])
            S.op("sp", lambda e: e.dma_start(out=C.MIXT[4 + h, :, b * 512:(b + 1) * 512], in_=res[rb][:]),
                 reads=[f"res{rb}"], writes=[f"MIXT_{4 + h}_{b}"], dma=True)

        head0_keys = ["vall"] + [f"qa0_{c}" for c in range(2)] + [f"qa0_{c}x" for c in range(2)] + \
            [f"ka0_{c}_{v}" for c in range(2) for v in range(2)] + [f"ka0_{c}_{v}x" for c in range(2) for v in range(2)]
        def emit_precast():
            precast_experts(C, 0, 8, head0_keys)

        for idx in range(U + LAG):
            if idx < U:
                stage_a(idx)
            if idx == 0:
                emit_precast()
            if idx - LAG >= 0:
                stage_b(idx - LAG)
        S.flush()


def phase_dftgen(C):
    nc, S, I = C.nc, C.S, C.I
    C.MDc = C.dscratch("MDc", [16, 128, 32, 256], BF16)
    C.MDs = C.dscratch("MDs", [16, 128, 32, 256], BF16)
    N = NFFT
    with ExitStack() as st:
        def sb(name, shape, dt):
            return st.enter_context(nc.sbuf_tensor(name, shape, dt))

        brow = sb("brow", [128, L], F32)
        c2b1 = sb("c2b1", [128, L], F32)
        acol = sb("acol", [128, NT], F32)
        a2s = sb("a2s", [128, NT], F32)
        tA = [sb(f"tA{i}", [128, L], I32) for i in range(2)]
        tB = [sb(f"tB{i}", [128, L], F32) for i in range(2)]
        tC = [sb(f"tC{i}", [128, L], I32) for i in range(2)]
        tD = [sb(f"tD{i}", [128, L], F32) for i in range(2)]
        outs = [sb(f"mos{i}", [128, L], BF16) for i in range(2)]
        outc = [sb(f"moc{i}", [128, L], BF16) for i in range(2)]

        S.op("pool", lambda e: e.iota(brow[:], [[1, L]], base=0, channel_multiplier=0,
                                      allow_small_or_imprecise_dtypes=True), writes=["brow"])
        S.op("pool", lambda e: e.iota(c2b1[:], [[2, L]], base=1, channel_multiplier=0,
                                      allow_small_or_imprecise_dtypes=True), writes=["c2b1"])
        S.op("pool", lambda e: e.iota(acol[:], [[128, NT]], base=0, channel_multiplier=1,
                                      allow_small_or_imprecise_dtypes=True), writes=["acol"])
        S.op("dve", lambda e: e.tensor_scalar(a2s[:], acol[:], 2.0, float(2 * N), ALU.mult, ALU.add),
             reads=["acol"], writes=["a2s"])
        mdc_v = C.MDc.rearrange("c p r j -> p c r j")
        mds_v = C.MDs.rearrange("c p r j -> p c r j")
        for rc in range(NT):
            ob = rc % 2
            S.op("dve", lambda e, rc=rc, ob=ob: e.tensor_scalar(tA[ob][:], brow[:], acol[:, rc:rc + 1], None, ALU.mult),
                 reads=["brow", "acol"], writes=[f"tA{ob}"])
            S.op("dve", lambda e, ob=ob: e.tensor_scalar(tA[ob][:], tA[ob][:], N - 1, None, ALU.bitwise_and),
                 reads=[f"tA{ob}"], writes=[f"tA{ob}"])
            S.op("dve", lambda e, ob=ob: e.scalar_tensor_tensor(out=tB[ob][:], in0=tA[ob][:], scalar=4.0, in1=c2b1[:],
                                                                op0=ALU.mult, op1=ALU.add),
                 reads=[f"tA{ob}", "c2b1"], writes=[f"tB{ob}"])
            S.op("dve", lambda e, rc=rc, ob=ob: e.tensor_scalar(tC[ob][:], tB[ob][:], a2s[:, rc:rc + 1], None, ALU.add),
                 reads=[f"tB{ob}", "a2s"], writes=[f"tC{ob}"])
            S.op("dve", lambda e, ob=ob: e.tensor_scalar(tC[ob][:], tC[ob][:], 4 * N - 1, None, ALU.bitwise_and),
                 reads=[f"tC{ob}"], writes=[f"tC{ob}"])
            S.op("act", lambda e, ob=ob: e.activation(out=outs[ob][:], in_=tC[ob][:], func=AF.Sin,
                                                      bias=float(-math.pi), scale=float(math.pi / (2 * N))),
                 reads=[f"tC{ob}"], writes=[f"mos{ob}"])
            S.op("act", lambda e, ob=ob: e.activation(out=tD[ob][:], in_=tC[ob][:], func=AF.Abs,
                                                      bias=float(-math.pi), scale=float(math.pi / (2 * N))),
                 reads=[f"tC{ob}"], writes=[f"tD{ob}"])
            S.op("act", lambda e, ob=ob: e.activation(out=outc[ob][:], in_=tD[ob][:], func=AF.Sin,
                                                      bias=float(math.pi / 2), scale=-1.0),
                 reads=[f"tD{ob}"], writes=[f"moc{ob}"])
            S.op("sp", lambda e, rc=rc, ob=ob: e.dma_start(out=mds_v[:, :, rc, :],
                                                           in_=outs[ob][:].rearrange("p (c j) -> p c j", j=256)),
                 reads=[f"mos{ob}"], writes=[f"MDs_{rc}"], dma=True)
            S.op("sp", lambda e, rc=rc, ob=ob: e.dma_start(out=mdc_v[:, :, rc, :],
                                                           in_=outc[ob][:].rearrange("p (c j) -> p c j", j=256)),
                 reads=[f"moc{ob}"], writes=[f"MDc_{rc}"], dma=True)
        S.flush()


MAGIC = 12582912.0
TWO_PI = 2.0 * math.pi


def emit_sin(S, eng_v, out, x, tmpa, tmpb, keys_in, key_out, ktmp):
    S.op(eng_v, lambda e: e.tensor_scalar(tmpa, x, 1.0 / TWO_PI, MAGIC, ALU.mult, ALU.add),
         reads=keys_in, writes=[ktmp + "a"])
    S.op(eng_v, lambda e: e.tensor_scalar(tmpa, tmpa, MAGIC, -TWO_PI, ALU.subtract, ALU.mult),
         reads=[ktmp + "a"], writes=[ktmp + "a"])
    S.op(eng_v, lambda e: e.tensor_tensor(out=tmpb, in0=tmpa, in1=x, op=ALU.add),
         reads=[ktmp + "a"] + list(keys_in), writes=[ktmp + "b"])
    S.op(eng_v, lambda e: e.tensor_scalar(tmpb, tmpb, math.pi, -math.pi, ALU.min, ALU.max),
         reads=[ktmp + "b"], writes=[ktmp + "b"])
    S.op("act", lambda e: e.activation(out=out, in_=tmpb, func=AF.Sin), reads=[ktmp + "b"], writes=[key_out])


def phase_filter(C):
    nc, S, I = C.nc, C.S, C.I
    N = NFFT
    C.GD = C.dscratch("GD", [2, 2, NT, 128, HW], F32)
    with ExitStack() as st:
        def sb(name, shape, dt):
            return st.enter_context(nc.sbuf_tensor(name, shape, dt))

        hid2 = sb("hid2", [64, L + 128], F32)
        S.op("dve", lambda e: e.memset(hid2[:, L:L + 128], 0.0), writes=["hid2pad"])
        w3 = sb("fw3", [64, 2048], F32)
        st_outer = st
        st = ExitStack()
        st.__enter__()

        def sbi(name, shape, dt):
            return st.enter_context(nc.sbuf_tensor(name, shape, dt))
        sb_outer = sb
        sb = sbi
        prow = sb("prow", [64, 1], F32)
        prow_i = sb("prow_i", [64, 1], I32)
        band = sb("band", [64, 1], F32)
        phase = sb("phase", [64, 1], F32)
        dpos = sb("dpos", [64, L], F32)
        zT = sb("zT", [64, L], F32)
        ta = sb("fta", [64, L], F32)
        tb = sb("ftb", [64, L], F32)
        ang = sb("ang", [64, L], F32)
        S.op("pool", lambda e: e.iota(prow_i[:], [[0, 1]], base=-1, channel_multiplier=1), writes=["prow_i"])
        S.op("pool", lambda e: e.iota(prow[:], [[0, 1]], base=0, channel_multiplier=1,
                                      allow_small_or_imprecise_dtypes=True), writes=["prow"])
        S.op("dve", lambda e: e.tensor_scalar(prow_i[:], prow_i[:], 15, None, ALU.bitwise_and),
             reads=["prow_i"], writes=["prow_i"])
        b0 = 1e-4
        bstep = (15.0 - 1e-4) / 15.0
        S.op("dve", lambda e: e.tensor_scalar(band[:], prow_i[:], float(bstep), float(b0), ALU.mult, ALU.add),
             reads=["prow_i"], writes=["band"])
        S.op("dve", lambda e: e.tensor_scalar(band[:], band[:], float(TWO_PI / L), None, ALU.mult),
             reads=["band"], writes=["band"])
        S.op("dve", lambda e: e.tensor_scalar(phase[:], prow[:], 16.5, float(math.pi / 2), ALU.is_ge, ALU.mult),
             reads=["prow"], writes=["phase"])
        S.op("dve", lambda e: e.tensor_scalar(phase[:], phase[:], float(math.pi / 2), None, ALU.add),
             reads=["phase"], writes=["phase"])
        S.op("pool", lambda e: e.iota(dpos[:], [[1, L]], base=0, channel_multiplier=0,
                                      allow_small_or_imprecise_dtypes=True), writes=["dpos"])
        S.op("dve", lambda e: e.tensor_scalar(ang[:], dpos[:], band[:, 0:1], phase[:, 0:1], ALU.mult, ALU.add),
             reads=["dpos", "band", "phase"], writes=["ang"])
        emit_sin(S, "dve", zT[:], ang[:], ta[:], tb[:], ["ang"], "zT", "ft")
        S.op("dve", lambda e: e.tensor_scalar(zT[0:1, :], dpos[0:1, :], float(1.0 / (L - 1)), None, ALU.mult),
             reads=["dpos", "zT"], writes=["zT"])

        w1 = sb("fw1", [33, 64], F32)
        w2 = sb("fw2", [64, 64], F32)
        fr = sb("ffr", [64, 1], F32)
        fb1 = sb("fb1", [64, 1], F32)
        fb2 = sb("fb2", [64, 1], F32)
        for t_, nm in ((w1, "filt_w1"), (w2, "filt_w2"), (w3, "filt_w3"), (fr, "filt_freq"), (fb1, "filt_b1"),
                       (fb2, "filt_b2")):
            S.op("sp", lambda e, t_=t_, nm=nm: e.dma_start(out=t_[:], in_=I[nm]), writes=[nm], dma=True)
        S.op("dve", lambda e: e.tensor_tensor(out=fb1[:], in0=fb1[:], in1=fr[:], op=ALU.mult),
             reads=["filt_b1", "filt_freq"], writes=["filt_b1"])
        S.op("dve", lambda e: e.tensor_tensor(out=fb2[:], in0=fb2[:], in1=fr[:], op=ALU.mult),
             reads=["filt_b2", "filt_freq"], writes=["filt_b2"])
        hid1 = sb("hid1", [64, L], F32)
        pre = ang
        for (src, wt, wk, kk, bias, bk, dst, dk) in ((zT, w1, "filt_w1", 33, fb1, "filt_b1", hid1, "hid1"),
                                                     (hid1, w2, "filt_w2", 64, fb2, "filt_b2", hid2, "hid2")):
            srck = "zT" if src is zT else "hid1"
            for blk in range(8):
                pb = 4 + blk % 2
                S.op("pe", lambda e, pb=pb, wt=wt, kk=kk, src=src, blk=blk: e.matmul(
                    C.ps[pb][0:64, :], lhsT=wt[0:kk, :], rhs=src[0:kk, blk * 512:(blk + 1) * 512],
                    start=True, stop=True), reads=[wk, srck], writes=[f"ps{pb}"])
                S.op("dve", lambda e, pb=pb, blk=blk, bias=bias: e.tensor_scalar(
                    pre[:, blk * 512:(blk + 1) * 512], C.ps[pb][0:64, :], fr[:, 0:1], bias[:, 0:1], ALU.mult, ALU.add),
                    reads=[f"ps{pb}", "filt_freq", bk], writes=["fpre"])
            emit_sin(S, "dve", dst[:, 0:L], pre[:], ta[:], tb[:], ["fpre"], dk, "ft")

        S.flush()
        st.__exit__(None, None, None)
        st = st_outer
        sb = sb_outer
        delta = sb("delta", [128, HW], F32)
        negt = sb("negt", [128, NT], F32)
        min_decay = math.log(1e-2) / 1.5
        max_decay = math.log(1e-2) / 0.3
        dstep = (max_decay - min_decay) / (HW - 1)
        S.op("pool", lambda e: e.iota(delta[:], [[1, HW]], base=0, channel_multiplier=0,
                                      allow_small_or_imprecise_dtypes=True), writes=["delta"])
        S.op("dve", lambda e: e.tensor_scalar(delta[:], delta[:], float(-dstep), float(-min_decay), ALU.mult, ALU.add),
             reads=["delta"], writes=["delta"])
        S.op("pool", lambda e: e.iota(negt[:], [[128, NT]], base=0, channel_multiplier=1,
                                      allow_small_or_imprecise_dtypes=True), writes=["negt"])
        S.op("dve", lambda e: e.tensor_scalar(negt[:], negt[:], float(-1.0 / (L - 1)), None, ALU.mult),
             reads=["negt"], writes=["negt"])
        S.op("dve", lambda e: e.tensor_scalar(negts[:], negt[:], float(-1.0 / (L - 1)), None, ALU.add),
             reads=["negt"], writes=["negts"])
        psic = sb("psic", [128, NT], F32)
        psis = sb("psis", [128, NT], F32)
        npsic = sb("npsic", [128, NT], F32)
        fidx = sb("fidx", [128, NT], F32)
        S.op("pool", lambda e: e.iota(fidx[:], [[256, NT]], base=1, channel_multiplier=2,
                                      allow_small_or_imprecise_dtypes=True), writes=["fidx"])
        S.op("act", lambda e: e.activation(out=psis[:], in_=fidx[:], func=AF.Sin, scale=float(math.pi / (2 * N))),
             reads=["fidx"], writes=["psis"])
        S.op("act", lambda e: e.activation(out=psic[:], in_=fidx[:], func=AF.Sin, scale=float(-math.pi / (2 * N)),
                                           bias=float(math.pi / 2)),
             reads=["fidx"], writes=["psic"])
        S.op("dve", lambda e: e.tensor_scalar(npsic[:], psic[:], -1.0, None, ALU.mult), reads=["psic"], writes=["npsic"])

        hsum = sb("hsum", [128, NT, HW], BF16)
        hdif = sb("hdif", [128, NT, HW], BF16)
        win = [sb(f"win{i}", [128, HW], F32) for i in range(2)]
        wins = [sb(f"wins{i}", [128, HW], F32) for i in range(2)]
        negts = sb("negts", [128, NT], F32)
        fw = [sb(f"fwd{i}", [128, HW], F32) for i in range(2)]
        bw = [sb(f"bwd{i}", [128, HW], F32) for i in range(2)]
        af = [sb(f"absf{i}", [128, HW], F32) for i in range(2)]
        ab = [sb(f"absb{i}", [128, HW], F32) for i in range(2)]
        rn2 = sb("rn2", [128, HW], F32)
        nrn2 = sb("nrn2", [128, HW], F32)
        mtc = [sb(f"mtc{i}", [128, NT, 256], BF16) for i in range(2)]
        mts = [sb(f"mts{i}", [128, NT, 256], BF16) for i in range(2)]
        g1 = sb("g1", [128, HW], F32)
        gre = [sb(f"gre{i}", [128, HW], F32) for i in range(2)]
        gim = [sb(f"gim{i}", [128, HW], F32) for i in range(2)]
        for n in range(2):
            for dc in range(NT):
                b = dc % 2
                S.op("act", lambda e, b=b, dc=dc: e.activation(out=win[b][:], in_=delta[:], func=AF.Exp,
                                                               scale=negt[:, dc:dc + 1]),
                     reads=["delta", "negt"], writes=[f"win{b}"])
                S.op("act", lambda e, b=b, dc=dc: e.activation(out=wins[b][:], in_=delta[:], func=AF.Exp,
                                                               scale=negts[:, dc:dc + 1]),
                     reads=["delta", "negts"], writes=[f"wins{b}"])
                S.op("pe", lambda e, dc=dc, n=n: e.matmul(C.ps[0][:], lhsT=hid2[:, dc * 128:(dc + 1) * 128],
                                                          rhs=w3[:, n * 512:(n + 1) * 512], start=True, stop=True),
                     reads=["hid2", "filt_w3"], writes=["ps0"])
                S.op("pe", lambda e, dc=dc, n=n: e.matmul(C.ps[1][:], lhsT=hid2[:, dc * 128 + 1:(dc + 1) * 128 + 1],
                                                          rhs=w3[:, 1024 + n * 512:1024 + (n + 1) * 512],
                                                          start=True, stop=True),
                     reads=["hid2", "hid2pad", "filt_w3"], writes=["ps1"])
                S.op("dve", lambda e, b=b: e.tensor_tensor(out=fw[b][:], in0=C.ps[0][:], in1=win[b][:], op=ALU.mult),
                     reads=["ps0", f"win{b}"], writes=[f"fwd{b}"])
                S.op("dve", lambda e, b=b: e.tensor_tensor(out=bw[b][:], in0=C.ps[1][:], in1=wins[b][:], op=ALU.mult),
                     reads=["ps1", f"wins{b}"], writes=[f"bwd{b}"])
                S.op("act", lambda e, b=b: e.activation(out=af[b][:], in_=fw[b][:], func=AF.Abs),
                     reads=[f"fwd{b}"], writes=[f"absf{b}"])
                S.op("act", lambda e, b=b: e.activation(out=ab[b][:], in_=bw[b][:], func=AF.Abs),
                     reads=[f"bwd{b}"], writes=[f"absb{b}"])
                S.op("pe", lambda e, b=b, dc=dc: e.matmul(C.ps[2][:], lhsT=C.ones_f[:], rhs=af[b][:],
                                                          start=(dc == 0), stop=False),
                     reads=["ones_f", f"absf{b}"], writes=["ps2"])
                S.op("pe", lambda e, b=b, dc=dc: e.matmul(C.ps[2][:], lhsT=C.ones_f[:], rhs=ab[b][:],
                                                          start=False, stop=(dc == NT - 1)),
                     reads=["ones_f", f"absb{b}"], writes=["ps2"])
                S.op("pool", lambda e, b=b, dc=dc: e.tensor_tensor(out=hsum[:, dc, :], in0=fw[b][:], in1=bw[b][:],
                                                                   op=ALU.add),
                     reads=[f"fwd{b}", f"bwd{b}"], writes=[f"hsum{dc}"])
                S.op("pool", lambda e, b=b, dc=dc: e.tensor_tensor(out=hdif[:, dc, :], in0=fw[b][:], in1=bw[b][:],
                                                                   op=ALU.subtract),
                     reads=[f"fwd{b}", f"bwd{b}"], writes=[f"hdif{dc}"])
            S.op("dve", lambda e: e.reciprocal(rn2[:], C.ps[2][:]), reads=["ps2"], writes=["rn2"])
            S.op("dve", lambda e: e.tensor_scalar(rn2[:], rn2[:], float(2.0 / N), None, ALU.mult),
                 reads=["rn2"], writes=["rn2"])
            S.op("dve", lambda e: e.tensor_scalar(nrn2[:], rn2[:], -1.0, None, ALU.mult),
                 reads=["rn2"], writes=["nrn2"])
            hs_keys = [f"hsum{dc}" for dc in range(NT)]
            hd_keys = [f"hdif{dc}" for dc in range(NT)]
            def fload(cc2):
                mb = cc2 % 2
                S.op("sp", lambda e: e.dma_start(out=mtc[mb][:], in_=C.MDc[cc2]), writes=[f"mtc{mb}"], dma=True)
                S.op("sp", lambda e: e.dma_start(out=mts[mb][:], in_=C.MDs[cc2]), writes=[f"mts{mb}"], dma=True)
            fload(0)
            for cc2 in range(16):
                mb = cc2 % 2
                if cc2 + 1 < 16:
                    fload(cc2 + 1)
                for sub in range(2):
                    fc = cc2 * 2 + sub
                    gb = fc % 2
                    pc, ps_ = (4, 5) if gb == 0 else (6, 7)
                    for (pb, mt, mk, hh, hk) in ((pc, mtc, "mtc", hsum, hs_keys), (ps_, mts, "mts", hdif, hd_keys)):
                        for rc in range(NT):
                            S.op("pe", lambda e, pb=pb, mt=mt, mb=mb, rc=rc, sub=sub, hh=hh: e.matmul(
                                C.ps[pb][:], lhsT=mt[mb][:, rc, sub * 128:(sub + 1) * 128], rhs=hh[:, rc, :],
                                start=(rc == 0), stop=(rc == NT - 1)),
                                reads=[f"{mk}{mb}", hk[rc]], writes=[f"ps{pb}"])
                    S.op("dve", lambda e, fc=fc, ps_=ps_: e.tensor_scalar(g1[:], C.ps[ps_][:], psis[:, fc:fc + 1], None, ALU.mult),
                         reads=[f"ps{ps_}", "psis"], writes=["g1"])
                    S.op("dve", lambda e, fc=fc, gb=gb, pc=pc: e.scalar_tensor_tensor(
                        out=gre[gb][:], in0=C.ps[pc][:], scalar=psic[:, fc:fc + 1], in1=g1[:], op0=ALU.mult, op1=ALU.add),
                        reads=[f"ps{pc}", "psic", "g1"], writes=[f"gre{gb}"])
                    S.op("dve", lambda e, gb=gb: e.tensor_tensor(out=gre[gb][:], in0=gre[gb][:], in1=rn2[:], op=ALU.mult),
                         reads=[f"gre{gb}", "rn2"], writes=[f"gre{gb}"])
                    S.op("dve", lambda e, fc=fc, ps_=ps_: e.tensor_scalar(g1[:], C.ps[ps_][:], npsic[:, fc:fc + 1], None, ALU.mult),
                         reads=[f"ps{ps_}", "npsic"], writes=["g1"])
                    S.op("dve", lambda e, fc=fc, gb=gb, pc=pc: e.scalar_tensor_tensor(
                        out=gim[gb][:], in0=C.ps[pc][:], scalar=psis[:, fc:fc + 1], in1=g1[:], op0=ALU.mult, op1=ALU.add),
                        reads=[f"ps{pc}", "psis", "g1"], writes=[f"gim{gb}"])
                    S.op("dve", lambda e, gb=gb: e.tensor_tensor(out=gim[gb][:], in0=gim[gb][:], in1=rn2[:], op=ALU.mult),
                         reads=[f"gim{gb}", "rn2"], writes=[f"gim{gb}"])
                    S.op("act", lambda e, n=n, fc=fc, gb=gb: e.dma_start(out=C.GD[n, 0, fc], in_=gre[gb][:]),
                         reads=[f"gre{gb}"], writes=[f"GD{n}0{fc}"], dma=True)
                    S.op("act", lambda e, n=n, fc=fc, gb=gb: e.dma_start(out=C.GD[n, 1, fc], in_=gim[gb][:]),
                         reads=[f"gim{gb}"], writes=[f"GD{n}1{fc}"], dma=True)
        S.flush()


def phase_conv(C):
    nc, S, I = C.nc, C.S, C.I
    C.Z1 = C.dscratch("Z1", [L, HW], F32)
    with ExitStack() as st:
        def sb(name, shape, dt):
            return st.enter_context(nc.sbuf_tensor(name, shape, dt))

        zin = sb("zin", [128, NT, HW], BF16)
        wre = sb("wre", [128, NT, HW], BF16)
        wim = sb("wim", [128, NT, HW], BF16)
        mtc = [sb(f"cmtc{i}", [128, NT, 256], BF16) for i in range(2)]
        mts = [sb(f"cmts{i}", [128, NT, 256], BF16) for i in range(2)]
        gre = [sb(f"cgre{i}", [128, HW], F32) for i in range(2)]
        gim = [sb(f"cgim{i}", [128, HW], F32) for i in range(2)]
        xc0 = sb("xc0", [128, HW], F32)
        xs0 = sb("xs0", [128, HW], F32)
        xc = [xc0, xc0]
        xs = [xs0, xs0]
        ta = sb("cta", [128, HW], F32)
        tb = sb("ctb", [128, HW], F32)
        tc_ = sb("ctc", [128, HW], F32)
        td = sb("ctd", [128, HW], F32)
        gate = [sb(f"gate{i}", [128, HW], F32) for i in range(2)]
        zf = [sb(f"zf{i}", [128, HW], F32) for i in range(2)]
        zo = [sb(f"zo{i}", [128, HW], F32) for i in range(2)]
        skipb = sb("skipb", [128, 2, HW], F32)
        ngb = sb("ngb", [128, HW], F32)
        sq = ta
        gs = sb("cgs", [128, 8], F32)
        zn = tb
        mxt = [sb(f"mxt{i}", [128, 4, 128], BF16) for i in range(2)]

        S.op("sp", lambda e: e.dma_start(out=zin[:], in_=C.VH.rearrange("(i p) n -> p i n", p=128)),
             writes=["zin"], dma=True)
        for n in range(2):
            S.op("sp", lambda e, n=n: e.dma_start(out=skipb[:, n, :], in_=bcast_rows(I["hyena_skip"][n:n + 1, :], 128)),
                 writes=[f"skipb{n}"], dma=True)
        S.op("sp", lambda e: e.dma_start(out=ngb[:], in_=bcast_rows(I["hyena_norm_g"], 128)), writes=["ngb"], dma=True)
        zin_keys = ["zin"] + [f"zin{t}" for t in range(NT)]
        for n in range(2):
            def cload(cc2):
                mb = cc2 % 2
                S.op("sp", lambda e: e.dma_start(out=mtc[mb][:], in_=C.MDc[cc2]), writes=[f"cmtc{mb}"], dma=True)
                S.op("sp", lambda e: e.dma_start(out=mts[mb][:], in_=C.MDs[cc2]), writes=[f"cmts{mb}"], dma=True)
            if n == 0:
                cload(0)
            for cc2 in range(16):
                mb = cc2 % 2
                cload((cc2 + 1) % 16)
                for sub in range(2):
                    fc = cc2 * 2 + sub
                    gb = fc % 2
                    S.op("sp", lambda e, n=n, fc=fc, gb=gb: e.dma_start(out=gre[gb][:], in_=C.GD[n, 0, fc]),
                         writes=[f"cgre{gb}"], dma=True)
                    S.op("sp", lambda e, n=n, fc=fc, gb=gb: e.dma_start(out=gim[gb][:], in_=C.GD[n, 1, fc]),
                         writes=[f"cgim{gb}"], dma=True)
                    pc, ps_ = (0, 1) if gb == 0 else (2, 3)
                    for (pb, mt, mk) in ((pc, mtc, "cmtc"), (ps_, mts, "cmts")):
                        for rc in range(NT):
                            S.op("pe", lambda e, pb=pb, mt=mt, mb=mb, rc=rc, sub=sub: e.matmul(
                                C.ps[pb][:], lhsT=mt[mb][:, rc, sub * 128:(sub + 1) * 128], rhs=zin[:, rc, :],
                                start=(rc == 0), stop=(rc == NT - 1)),
                                reads=[f"{mk}{mb}"] + zin_keys, writes=[f"ps{pb}"])
                    S.op("act", lambda e, gb=gb, pc=pc: e.copy(out=xc[gb][:], in_=C.ps[pc][:]),
                         reads=[f"ps{pc}"], writes=["xc0"])
                    S.op("act", lambda e, gb=gb, ps_=ps_: e.copy(out=xs[gb][:], in_=C.ps[ps_][:]),
                         reads=[f"ps{ps_}"], writes=["xs0"])
                    S.op("dve", lambda e, gb=gb: e.tensor_tensor(out=ta[:], in0=xc[gb][:], in1=gre[gb][:], op=ALU.mult),
                         reads=["xc0", f"cgre{gb}"], writes=["cta"])
                    S.op("dve", lambda e, gb=gb: e.tensor_tensor(out=tb[:], in0=xs[gb][:], in1=gim[gb][:], op=ALU.mult),
                         reads=["xs0", f"cgim{gb}"], writes=["ctb"])
                    S.op("dve", lambda e, fc=fc: e.tensor_tensor(out=wre[:, fc, :], in0=ta[:], in1=tb[:], op=ALU.add),
                         reads=["cta", "ctb"], writes=[f"wre{fc}"])
                    S.op("pool", lambda e, gb=gb: e.tensor_tensor(out=tc_[:], in0=xs[gb][:], in1=gre[gb][:], op=ALU.mult),
                         reads=["xs0", f"cgre{gb}"], writes=["ctc"])
                    S.op("pool", lambda e, gb=gb: e.tensor_tensor(out=td[:], in0=xc[gb][:], in1=gim[gb][:], op=ALU.mult),
                         reads=["xc0", f"cgim{gb}"], writes=["ctd"])
                    S.op("pool", lambda e, fc=fc: e.tensor_tensor(out=wim[:, fc, :], in0=tc_[:], in1=td[:],
                                                                  op=ALU.subtract),
                         reads=["ctc", "ctd"], writes=[f"wim{fc}"])
            wre_keys = [f"wre{f}" for f in range(NT)]
            wim_keys = [f"wim{f}" for f in range(NT)]
            for cc2 in range(16):
                mb = cc2 % 2
                if not (n == 1 and cc2 == 15):
                    cload((cc2 + 1) % 16)
                for sub in range(2):
                    tcx = cc2 * 2 + sub
                    gb = tcx % 2
                    gsrc = C.U[n]
                    zsrc = C.U[2] if n == 0 else C.Z1
                    S.op("sp", lambda e, gb=gb, gsrc=gsrc, tcx=tcx: e.dma_start(
                        out=gate[gb][:], in_=gsrc[tcx * 128:(tcx + 1) * 128, :]), writes=[f"gate{gb}"], dma=True)
                    S.op("sp", lambda e, gb=gb, zsrc=zsrc, tcx=tcx: e.dma_start(
                        out=zf[gb][:], in_=zsrc[tcx * 128:(tcx + 1) * 128, :]),
                        reads=([f"Z1_{tcx}"] if n == 1 else []), writes=[f"zf{gb}"], dma=True)
                    pb = 4 + gb
                    for fcx in range(NT):
                        S.op("pe", lambda e, pb=pb, mb=mb, fcx=fcx, sub=sub: e.matmul(
                            C.ps[pb][:], lhsT=mtc[mb][:, fcx, sub * 128:(sub + 1) * 128], rhs=wre[:, fcx, :],
                            start=(fcx == 0), stop=False),
                            reads=[f"cmtc{mb}", wre_keys[fcx]], writes=[f"ps{pb}"])
                        S.op("pe", lambda e, pb=pb, mb=mb, fcx=fcx, sub=sub: e.matmul(
                            C.ps[pb][:], lhsT=mts[mb][:, fcx, sub * 128:(sub + 1) * 128], rhs=wim[:, fcx, :],
                            start=False, stop=(fcx == NT - 1)),
                            reads=[f"cmts{mb}", wim_keys[fcx]], writes=[f"ps{pb}"])
                    S.op("dve", lambda e, gb=gb, n=n: e.tensor_tensor(out=zo[gb][:], in0=zf[gb][:], in1=skipb[:, n, :],
                                                                     op=ALU.mult),
                         reads=[f"zf{gb}", f"skipb{n}"], writes=[f"zo{gb}"])
                    S.op("dve", lambda e, gb=gb, pb=pb: e.tensor_tensor(out=zo[gb][:], in0=zo[gb][:], in1=C.ps[pb][:],
                                                                       op=ALU.add),
                         reads=[f"zo{gb}", f"ps{pb}"], writes=[f"zo{gb}"])
                    S.op("dve", lambda e, gb=gb: e.tensor_tensor(out=zo[gb][:], in0=zo[gb][:], in1=gate[gb][:],
                                                                op=ALU.mult),
                         reads=[f"zo{gb}", f"gate{gb}"], writes=[f"zo{gb}"])
                    if n == 0:
                        S.op("act", lambda e, gb=gb, tcx=tcx: e.dma_start(out=C.Z1[tcx * 128:(tcx + 1) * 128, :],
                                                                          in_=zo[gb][:]),
                             reads=[f"zo{gb}"], writes=[f"Z1_{tcx}"], dma=True)
                        S.op("act", lambda e, gb=gb, tcx=tcx: e.copy(out=zin[:, tcx, :], in_=zo[gb][:]),
                             reads=[f"zo{gb}"], writes=[f"zin{tcx}"])
                    else:
                        S.op("pool", lambda e, gb=gb: e.tensor_tensor(out=sq[:], in0=zo[gb][:], in1=zo[gb][:], op=ALU.mult),
                             reads=[f"zo{gb}"], writes=["cta"])
                        S.op("dve", lambda e: e.reduce_sum(out=gs[:], in_=sq[:].rearrange("p (g c) -> p g c", g=8),
                                                           axis=AX.X),
                             reads=["cta"], writes=["cgs"])
                        S.op("act", lambda e: e.activation(out=gs[:], in_=gs[:], func=AF.Sqrt, bias=float(EPS),
                                                           scale=1.0 / 64.0), reads=["cgs"], writes=["cgs"])
                        S.op("dve", lambda e: e.reciprocal(gs[:], gs[:]), reads=["cgs"], writes=["cgs"])
                        S.op("dve", lambda e, gb=gb: e.tensor_tensor(
                            out=zn[:].rearrange("p (g c) -> p g c", g=8),
                            in0=zo[gb][:].rearrange("p (g c) -> p g c", g=8),
                            in1=gs[:].unsqueeze(2).to_broadcast([128, 8, 64]), op=ALU.mult),
                            reads=[f"zo{gb}", "cgs"], writes=["ctb"])
                        S.op("pool", lambda e: e.tensor_tensor(out=zn[:], in0=zn[:], in1=ngb[:], op=ALU.mult),
                             reads=["ctb", "ngb"], writes=["ctb"])
                        pt = 6 + gb
                        for k in range(4):
                            S.op("pe", lambda e, pt=pt, k=k: e.transpose(
                                out=C.ps[pt][:, k * 128:(k + 1) * 128], in_=zn[:, k * 128:(k + 1) * 128],
                                identity=C.ident[:]), reads=["ctb", "ident"], writes=[f"ps{pt}"])
                        S.op("act", lambda e, pt=pt, gb=gb: e.copy(out=mxt[gb][:],
                                                                   in_=C.ps[pt][:].rearrange("p (k t) -> p k t", k=4)),
                             reads=[f"ps{pt}"], writes=[f"mxt{gb}"])
                        S.op("act", lambda e, gb=gb, tcx=tcx: e.dma_start(
                            out=C.MIXT[0:4, :, tcx * 128:(tcx + 1) * 128].rearrange("k p t -> p k t"), in_=mxt[gb][:]),
                            reads=[f"mxt{gb}"], writes=[f"MIXTh_{tcx}"], dma=True)
        S.flush()


def phase_oproj(C):
    nc, S, I = C.nc, C.S, C.I
    C.X2 = C.dscratch("X2", [L, D], F32)
    C.H2 = C.dscratch("H2", [L, D], BF16)
    C.aff = C.stack.enter_context(nc.sbuf_tensor("aff", [128, NT, NE], F32))
    with ExitStack() as st:
        def sb(name, shape, dt):
            return st.enter_context(nc.sbuf_tensor(name, shape, dt))

        mix = sb("mix", [128, 8, L], BF16)
        wst = sb("owst", [128, 8, 512], F32)
        wbf = sb("owbf", [128, 8, D], BF16)
        g32 = sb("og32", [128, D], F32)
        wr = sb("owr", [128, 8, NE], F32)
        xt = [sb(f"oxt{i}", [128, D], F32) for i in range(2)]
        x2 = [sb(f"ox2{i}", [128, D], F32) for i in range(2)]
        h2 = [sb(f"oh2{i}", [128, D], F32) for i in range(2)]
        h2b = [sb(f"oh2b{i}", [128, D], BF16) for i in range(2)]
        h2T = sb("oh2T", [128, 8, 128], F32)
        junk = sb("ojunk", [128, D], F32)
        ss = [sb(f"oss{i}", [128, 1], F32) for i in range(2)]
        rstd = [sb(f"orstd{i}", [128, 1], F32) for i in range(2)]
        lg = sb("olg", [128, NE], F32)
        mx = sb("omx", [128, 1], F32)
        es = sb("oes", [128, 1], F32)

        for m in range(8):
            S.op("sp", lambda e, m=m: e.dma_start(out=mix[:, m, :], in_=C.MIXT[m]), writes=[f"mix{m}"], dma=True)
        mix_keys = [f"mix{m}" for m in range(8)]
        wv = I["w_out"].rearrange("(k p) n -> p k n", p=128)
        for half in range(2):
            S.op("sp", lambda e, half=half: e.dma_start(out=wst[:], in_=wv[:, :, half * 512:(half + 1) * 512]),
                 writes=["owst"], dma=True)
            S.op("act", lambda e, half=half: e.copy(out=wbf[:, :, half * 512:(half + 1) * 512], in_=wst[:]),
                 reads=["owst"], writes=[f"owbf{half}"])
        S.op("sp", lambda e: e.dma_start(out=g32[:], in_=bcast_rows(I["ffn_norm_g"], 128)), writes=["og32"], dma=True)
        S.op("dve", lambda e: e.tensor_scalar(g32[:], g32[:], 32.0, None, ALU.mult), reads=["og32"], writes=["og32"])
        S.op("sp", lambda e: e.dma_start(out=wr[:], in_=I["w_router"].rearrange("(k p) n -> p k n", p=128)),
             writes=["owr"], dma=True)
        for i in range(NT):
            b = i % 2
            S.op("sp", lambda e, i=i, b=b: e.dma_start(out=xt[b][:], in_=I["x"][i * 128:(i + 1) * 128, :]),
                 writes=[f"oxt{b}"], dma=True)
            for half in range(2):
                pb = half
                for m in range(8):
                    S.op("pe", lambda e, pb=pb, m=m, i=i, half=half: e.matmul(
                        C.ps[pb][:], lhsT=mix[:, m, i * 128:(i + 1) * 128], rhs=wbf[:, m, half * 512:(half + 1) * 512],
                        start=(m == 0), stop=(m == 7)), reads=[mix_keys[m], f"owbf{half}"], writes=[f"ps{pb}"])
                S.op("dve", lambda e, pb=pb, b=b, half=half: e.tensor_tensor(
                    out=x2[b][:, half * 512:(half + 1) * 512], in0=C.ps[pb][:], in1=xt[b][:, half * 512:(half + 1) * 512],
                    op=ALU.add), reads=[f"ps{pb}", f"oxt{b}"], writes=[f"ox2{b}_{half}"])
            x2k = [f"ox2{b}_0", f"ox2{b}_1"]
            S.op("pool", lambda e, i=i, b=b: e.dma_start(out=C.X2[i * 128:(i + 1) * 128, :], in_=x2[b][:]),
                 reads=x2k, writes=[f"X2_{i}"], dma=True)
            S.op("dve", lambda e, b=b: e.memset(ss[b][:], 0.0), writes=[f"oss{b}"])
            S.op("act", lambda e, b=b: e.activation(out=junk[:], in_=x2[b][:], func=AF.Square, accum_out=ss[b][:]),
                 reads=x2k + [f"oss{b}"], writes=[f"oss{b}", "ojunk"])
            S.op("act", lambda e, b=b: e.activation(out=ss[b][:], in_=ss[b][:], func=AF.Sqrt, bias=float(D * EPS),
                                                    scale=1.0), reads=[f"oss{b}"], writes=[f"oss{b}"])
            S.op("dve", lambda e, b=b: e.reciprocal(rstd[b][:], ss[b][:]), reads=[f"oss{b}"], writes=[f"orstd{b}"])
            S.op("dve", lambda e, b=b: e.scalar_tensor_tensor(out=h2[b][:], in0=x2[b][:], scalar=rstd[b][:, 0:1],
                                                              in1=g32[:], op0=ALU.mult, op1=ALU.mult),
                 reads=x2k + [f"orstd{b}", "og32"], writes=[f"oh2{b}"])
            S.op("act", lambda e, b=b: e.copy(out=h2b[b][:], in_=h2[b][:]), reads=[f"oh2{b}"], writes=[f"oh2b{b}"])
            S.op("pool", lambda e, i=i, b=b: e.dma_start(out=C.H2[i * 128:(i + 1) * 128, :], in_=h2b[b][:]),
                 reads=[f"oh2b{b}"], writes=[f"H2_{i}"], dma=True)
            for half in range(2):
                pb = 2 + half
                for kk in range(4):
                    k = half * 4 + kk
                    S.op("pe", lambda e, b=b, k=k, kk=kk, pb=pb: e.transpose(
                        out=C.ps[pb][:, kk * 128:(kk + 1) * 128], in_=h2[b][:, k * 128:(k + 1) * 128],
                        identity=C.ident[:]), reads=[f"oh2{b}", "ident"], writes=[f"ps{pb}"])
                eng = "act" if half == 0 else "dve"
                if eng == "act":
                    S.op("act", lambda e, half=half, pb=pb: e.copy(
                        out=h2T[:, half * 4:(half + 1) * 4, :], in_=C.ps[pb][:].rearrange("p (k t) -> p k t", k=4)),
                        reads=[f"ps{pb}"], writes=[f"oh2T{half}"])
                else:
                    S.op("dve", lambda e, half=half, pb=pb: e.tensor_copy(
                        out=h2T[:, half * 4:(half + 1) * 4, :], in_=C.ps[pb][:].rearrange("p (k t) -> p k t", k=4)),
                        reads=[f"ps{pb}"], writes=[f"oh2T{half}"])
            pl = 4 + b
            for k in range(8):
                S.op("pe", lambda e, pl=pl, k=k: e.matmul(C.ps[pl][:, 0:NE], lhsT=h2T[:, k, :], rhs=wr[:, k, :],
                                                          start=(k == 0), stop=(k == 7)),
                     reads=[f"oh2T{k // 4}", "owr"], writes=[f"ps{pl}"])
            S.op("dve", lambda e, pl=pl: e.tensor_copy(lg[:], C.ps[pl][:, 0:NE]), reads=[f"ps{pl}"], writes=["olg"])
            S.op("dve", lambda e: e.reduce_max(out=mx[:], in_=lg[:], axis=AX.X), reads=["olg"], writes=["omx"])
            S.op("dve", lambda e: e.tensor_scalar(mx[:], mx[:], -1.0, None, ALU.mult), reads=["omx"], writes=["omx"])
            S.op("dve", lambda e: e.memset(es[:], 0.0), writes=["oes"])
            S.op("act", lambda e: e.activation(out=lg[:], in_=lg[:], func=AF.Exp, bias=mx[:, 0:1], scale=1.0,
                                               accum_out=es[:]), reads=["olg", "omx", "oes"], writes=["olg", "oes"])
            S.op("dve", lambda e: e.reciprocal(es[:], es[:]), reads=["oes"], writes=["oes"])
            S.op("dve", lambda e, i=i: e.tensor_scalar(C.aff[:, i, :], lg[:], es[:, 0:1], None, ALU.mult),
                 reads=["olg", "oes"], writes=[f"aff{i}"])
        S.flush()


def phase_topk(C):
    nc, S, I = C.nc, C.S, C.I
    C.idx_i = C.stack.enter_context(nc.sbuf_tensor("idx_i", [128, NE * 4], I32))
    C.gsel = C.stack.enter_context(nc.sbuf_tensor("gsel", [128, NE * 4], F32))
    with ExitStack() as st:
        def sb(name, shape, dt):
            return st.enter_context(nc.sbuf_tensor(name, shape, dt))

        maskT = sb("tmaskT", [128, NT, NE], F32)
        pre = sb("tpre", [128, NT, NE], F32)
        slot = sb("tslot", [128, NT, NE], F32)
        tri = sb("ttri", [128, 128], F32)
        jio = sb("tjio", [128, CAP], F32)
        tg = sb("ttg", [128, NT, NE, 5], BF16)
        tokp = sb("ttokp", [128, NT], F32)
        toki = sb("ttoki", [128, NT], F32)
        a1 = sb("ta1", [128, NT, NE], BF16)
        a2 = sb("ta2", [128, NT, NE], BF16)
        a3 = sb("ta3", [128, NT, NE], BF16)
        rr = sb("trr", [128, NT, NE], F32)
        af32 = sb("taf32", [128, NT, NE], F32)
        ssel = [sb(f"tssel{i}", [128, CAP], BF16) for i in range(4)]
        ig = sb("tig", [128, 5], F32)
        idf = sb("tidf", [128, 1], F32)

        thr = sb("tthr", [128, NE], F32)
        cmpt = sb("tcmp", [128, NT, NE], F32)
        cnt = sb("tcnt", [128, NE], F32)
        ge = sb("tge", [128, NE], F32)
        S.op("dve", lambda e: e.memset(thr[:], 0.5), writes=["tthr"])
        NIT = 24
        for it in range(NIT):
            pb = it % 2
            step = 0.5 ** (it + 1)
            S.op("dve", lambda e: e.tensor_tensor(out=cmpt[:], in0=C.aff[:],
                                                  in1=thr[:].unsqueeze(1).to_broadcast([128, NT, NE]), op=ALU.is_gt),
                 reads=["tthr"], writes=["tcmp"])
            S.op("dve", lambda e: e.reduce_sum(out=cnt[:], in_=cmpt[:].rearrange("p i e -> p e i"), axis=AX.X),
                 reads=["tcmp"], writes=["tcnt"])
            S.op("pe", lambda e, pb=pb: e.matmul(C.ps[pb][:, 0:NE], lhsT=C.ones_f[:], rhs=cnt[:], start=True, stop=True),
                 reads=["ones_f", "tcnt"], writes=[f"ps{pb}"])
            S.op("dve", lambda e, pb=pb: e.tensor_scalar(ge[:], C.ps[pb][:, 0:NE], float(CAP) - 0.5, -0.5, ALU.is_gt, ALU.add),
                 reads=[f"ps{pb}"], writes=["tge"])
            S.op("dve", lambda e, step=step: e.scalar_tensor_tensor(out=thr[:], in0=ge[:], scalar=float(step), in1=thr[:],
                                                                    op0=ALU.mult, op1=ALU.add),
                 reads=["tge", "tthr"], writes=["tthr"])
        S.op("dve", lambda e: e.tensor_scalar(thr[:], thr[:], float(-(0.5 ** (NIT + 1))), None, ALU.add),
             reads=["tthr"], writes=["tthr"])
        S.op("dve", lambda e: e.tensor_tensor(out=maskT[:], in0=C.aff[:],
                                              in1=thr[:].unsqueeze(1).to_broadcast([128, NT, NE]), op=ALU.is_gt),
             reads=["tthr"], writes=[f"tmaskT{i}" for i in range(NT)])
        S.op("pool", lambda e: e.memset(pre[:, 0, :], 0.0), writes=["tpre0"])
        for i in range(1, NT):
            S.op("pool", lambda e, i=i: e.tensor_tensor(out=pre[:, i, :], in0=pre[:, i - 1, :], in1=maskT[:, i - 1, :],
                                                        op=ALU.add),
                 reads=[f"tpre{i - 1}", f"tmaskT{i - 1}"], writes=[f"tpre{i}"])
        S.op("pool", lambda e: e.iota(tri[:], [[1, 128]], base=0, channel_multiplier=-1,
                                      allow_small_or_imprecise_dtypes=True), writes=["ttri"])
        S.op("dve", lambda e: e.tensor_scalar(tri[:], tri[:], 0.0, None, ALU.is_gt), reads=["ttri"], writes=["ttri"])
        for i in range(NT):
            pb = 2 + i % 2
            S.op("pe", lambda e, i=i, pb=pb: e.matmul(C.ps[pb][:, 0:NE], lhsT=tri[:], rhs=maskT[:, i, :],
                                                      start=True, stop=False),
                 reads=["ttri", f"tmaskT{i}"], writes=[f"ps{pb}"])
            S.op("pe", lambda e, i=i, pb=pb: e.matmul(C.ps[pb][:, 0:NE], lhsT=C.ones_f[:], rhs=pre[:, i, :],
                                                      start=False, stop=True),
                 reads=["ones_f", f"tpre{i}"], writes=[f"ps{pb}"])
            S.op("dve", lambda e, i=i, pb=pb: e.scalar_tensor_tensor(out=slot[:, i, :], in0=C.ps[pb][:, 0:NE], scalar=1.0,
                                                                     in1=maskT[:, i, :], op0=ALU.add, op1=ALU.mult),
                 reads=[f"ps{pb}", f"tmaskT{i}"], writes=[f"tslot{i}"])
            S.op("dve", lambda e, i=i: e.tensor_scalar(slot[:, i, :], slot[:, i, :], -1.0, None, ALU.add),
                 reads=[f"tslot{i}"], writes=[f"tslot{i}"])
        S.op("pool", lambda e: e.iota(jio[:], [[1, CAP]], base=0, channel_multiplier=0,
                                      allow_small_or_imprecise_dtypes=True), writes=["tjio"])
        S.op("pool", lambda e: e.iota(toki[:], [[1, NT]], base=0, channel_multiplier=0,
                                      allow_small_or_imprecise_dtypes=True), writes=["ttoki"])
        S.op("pool", lambda e: e.iota(tokp[:], [[0, NT]], base=0, channel_multiplier=1,
                                      allow_small_or_imprecise_dtypes=True), writes=["ttokp"])
        aff_keys = []
        S.op("dve", lambda e: e.tensor_copy(tg[:, :, :, 0], toki[:].unsqueeze(2).to_broadcast([128, NT, NE])),
             reads=["ttoki"], writes=["ttg0"])
        S.op("dve", lambda e: e.tensor_copy(tg[:, :, :, 1], tokp[:].unsqueeze(2).to_broadcast([128, NT, NE])),
             reads=["ttokp"], writes=["ttg1"])
        S.op("dve", lambda e: e.tensor_copy(a1[:], C.aff[:]), writes=["ta1"])
        S.op("dve", lambda e: e.tensor_copy(af32[:], a1[:]), reads=["ta1"], writes=["taf32"])
        S.op("dve", lambda e: e.tensor_tensor(out=rr[:], in0=C.aff[:], in1=af32[:], op=ALU.subtract),
             reads=["taf32"], writes=["trr"])
        S.op("dve", lambda e: e.tensor_copy(a2[:], rr[:]), reads=["trr"], writes=["ta2"])
        S.op("dve", lambda e: e.tensor_copy(af32[:], a2[:]), reads=["ta2"], writes=["taf32"])
        S.op("dve", lambda e: e.tensor_tensor(out=rr[:], in0=rr[:], in1=af32[:], op=ALU.subtract),
             reads=["taf32", "trr"], writes=["trr"])
        S.op("dve", lambda e: e.tensor_copy(a3[:], rr[:]), reads=["trr"], writes=["ta3"])
        for c_, (a_, k_) in enumerate(((a1, "ta1"), (a2, "ta2"), (a3, "ta3"))):
            S.op("pool", lambda e, c_=c_, a_=a_: e.tensor_copy(tg[:, :, :, 2 + c_], a_[:]),
                 reads=[k_], writes=[f"ttg{2 + c_}"])
        tgk = [f"ttg{c_}" for c_ in range(5)]
        sc = 0
        for ex in range(NE):
            base = 0 if ex % 2 == 0 else 4
            for i in range(NT):
                sbk = sc % 4
                sc += 1
                S.op("dve", lambda e, i=i, ex=ex, sbk=sbk: e.tensor_scalar(ssel[sbk][:], jio[:], slot[:, i, ex:ex + 1], None,
                                                                          ALU.is_equal),
                     reads=["tjio", f"tslot{i}"], writes=[f"tssel{sbk}"])
                for jc in range(4):
                    S.op("pe", lambda e, i=i, ex=ex, sbk=sbk, jc=jc, base=base: e.matmul(
                        C.ps[base + jc][:, 0:5], lhsT=ssel[sbk][:, jc * 128:(jc + 1) * 128], rhs=tg[:, i, ex, :],
                        start=(i == 0), stop=(i == NT - 1)),
                        reads=[f"tssel{sbk}"] + tgk, writes=[f"ps{base + jc}"])
            for jc in range(4):
                col = ex * 4 + jc
                S.op("act", lambda e, jc=jc, base=base: e.copy(out=ig[:], in_=C.ps[base + jc][:, 0:5]),
                     reads=[f"ps{base + jc}"], writes=["tig"])
                S.op("dve", lambda e: e.scalar_tensor_tensor(out=idf[:], in0=ig[:, 0:1], scalar=128.0, in1=ig[:, 1:2],
                                                             op0=ALU.mult, op1=ALU.add),
                     reads=["tig"], writes=["tidf"])
                S.op("dve", lambda e, col=col: e.tensor_copy(C.idx_i[:, col:col + 1], idf[:]),
                     reads=["tidf"], writes=[f"idx{col}"])
                S.op("dve", lambda e, col=col: e.tensor_tensor(out=C.gsel[:, col:col + 1], in0=ig[:, 2:3], in1=ig[:, 3:4],
                                                               op=ALU.add),
                     reads=["tig"], writes=[f"gsel{col}"])
                S.op("dve", lambda e, col=col: e.tensor_tensor(out=C.gsel[:, col:col + 1], in0=C.gsel[:, col:col + 1],
                                                               in1=ig[:, 4:5], op=ALU.add),
                     reads=["tig", f"gsel{col}"], writes=[f"gsel{col}"])
        S.flush()


def phase_moe(C):
    nc, S, I = C.nc, C.S, C.I
    with ExitStack() as st:
        def sb(name, shape, dt):
            return st.enter_context(nc.sbuf_tensor(name, shape, dt))

        xg = [sb(f"xg{j}", [128, D], BF16) for j in range(4)]
        xgT = sb("xgT", [128, 8, CAP], BF16)
        hT = sb("hT", [128, NFC, CAP], BF16)
        NWB = 4
        PF = NWB - 1
        wgt = [sb(f"wgt{i}", [128, 8, 256], BF16) for i in range(NWB)]
        wut = [sb(f"wut{i}", [128, 8, 256], BF16) for i in range(NWB)]
        wdt = [sb(f"wdt{i}", [128, NFC, D], BF16) for i in range(2)]
        sa = [sb(f"sa{i}", [128, CAP], F32) for i in range(2)]
        yy = [sb(f"yy{i}", [128, D], F32) for i in range(4)]
        psb = [C.ps[6][:].bitcast(BF16), C.ps[7][:].bitcast(BF16)]
        NFB = NFC // 2
        blocks = [(ex, fb) for ex in range(NE) for fb in range(NFB)]

        NPRE = getattr(C, "NPRE", 0)

        def emit_wload(bi):
            ex, fb = blocks[bi]
            wb = bi % NWB
            if ex < NPRE:
                q_, wg_src, wu_src = "sp", C.WGB[ex], C.WUB[ex]
            else:
                q_, wg_src, wu_src = "pool", I["w_gate"][ex], I["w_up"][ex]
            wgv = wg_src.rearrange("(k p) f -> p k f", p=128)
            wuv = wu_src.rearrange("(k p) f -> p k f", p=128)
            S.op(q_, lambda e: e.dma_start(out=wgt[wb][:], in_=wgv[:, :, fb * 256:(fb + 1) * 256]),
                 writes=[f"wgt{wb}"], dma=True)
            S.op(q_, lambda e: e.dma_start(out=wut[wb][:], in_=wuv[:, :, fb * 256:(fb + 1) * 256]),
                 writes=[f"wut{wb}"], dma=True)

        def emit_wd(ex):
            wdb = ex % 2
            if ex < NPRE:
                q_, src = "sp", C.WDB[ex]
            else:
                q_, src = "pool", I["w_down"][ex]
            wdv = src.rearrange("(c p) d -> p c d", p=128)
            for hh in range(2):
                S.op(q_, lambda e, hh=hh: e.dma_start(
                    out=wdt[wdb][:, hh * 11:(hh + 1) * 11, :], in_=wdv[:, hh * 11:(hh + 1) * 11, :]),
                    writes=[f"wdt{wdb}_{hh}"], dma=True)

        def emit_gather(ex):
            for jc in range(4):
                col = ex * 4 + jc
                S.op("pool", lambda e, jc=jc, col=col: e.indirect_dma_start(
                    out=xg[jc][:], out_offset=None, in_=C.H2,
                    in_offset=bass.IndirectOffsetOnAxis(ap=C.idx_i[:, col:col + 1], axis=0)),
                    writes=[f"xg{jc}"], dma=True)

        pending_sc = []

        def flush_scatters(n):
            for _ in range(min(n, len(pending_sc))):
                yb, col = pending_sc.pop(0)
                S.op("pool", lambda e, yb=yb, col=col: e.indirect_dma_start(
                    out=C.X2, out_offset=bass.IndirectOffsetOnAxis(ap=C.idx_i[:, col:col + 1], axis=0),
                    in_=yy[yb][:], in_offset=None, compute_op=ALU.add, oob_is_err=True),
                    reads=[f"yy{yb}_0", f"yy{yb}_1"], writes=["X2"], dma=True)

        emit_gather(0)
        for bi in range(PF):
            emit_wload(bi)
        emit_wd(0)
        gu = 0
        yc = 0
        bi = 0
        for ex in range(NE):
            for jc in range(4):
                pb = jc % 2
                for k in range(8):
                    S.op("pe", lambda e, jc=jc, k=k, pb=pb: e.transpose(
                        out=psb[pb][:, k * 128:(k + 1) * 128], in_=xg[jc][:, k * 128:(k + 1) * 128],
                        identity=C.identb[:]), reads=[f"xg{jc}", "identb"], writes=[f"ps{6 + pb}"])
                if jc % 2 == 0:
                    S.op("act", lambda e, jc=jc, pb=pb: e.copy(out=xgT[:, :, jc * 128:(jc + 1) * 128],
                                                               in_=psb[pb][:].rearrange("p (k t) -> p k t", k=8)),
                         reads=[f"ps{6 + pb}"], writes=[f"xgT{jc}"])
                else:
                    S.op("dve", lambda e, jc=jc, pb=pb: e.tensor_copy(out=xgT[:, :, jc * 128:(jc + 1) * 128],
                                                                      in_=psb[pb][:].rearrange("p (k t) -> p k t", k=8)),
                         reads=[f"ps{6 + pb}"], writes=[f"xgT{jc}"])
            if ex + 1 < NE:
                emit_gather(ex + 1)
                emit_wd(ex + 1)
            xk = [f"xgT{jc}" for jc in range(4)]
            wdb = ex % 2
            for fb in range(NFB):
                if bi + PF < len(blocks):
                    emit_wload(bi + PF)
                if fb in (1, 2, 3, 4):
                    flush_scatters(1)
                wb = bi % NWB
                bi += 1
                for sub in range(2):
                    fc = fb * 2 + sub
                    g2 = gu % 2
                    gu += 1
                    pa, pu = (0, 1) if g2 == 0 else (2, 3)
                    for (pb, wt, wk) in ((pa, wgt, "wgt"), (pu, wut, "wut")):
                        for k in range(8):
                            S.op("pe", lambda e, pb=pb, wt=wt, wb=wb, k=k, sub=sub: e.matmul(
                                C.ps[pb][:], lhsT=wt[wb][:, k, sub * 128:(sub + 1) * 128], rhs=xgT[:, k, :],
                                start=(k == 0), stop=(k == 7)), reads=[f"{wk}{wb}"] + xk, writes=[f"ps{pb}"])
                    S.op("act", lambda e, g2=g2, pa=pa: e.activation(out=sa[g2][:], in_=C.ps[pa][:], func=AF.Silu),
                         reads=[f"ps{pa}"], writes=[f"sa{g2}"])
                    S.op("dve", lambda e, g2=g2, pu=pu, fc=fc: e.tensor_tensor(out=hT[:, fc, :], in0=sa[g2][:],
                                                                              in1=C.ps[pu][:], op=ALU.mult),
                         reads=[f"sa{g2}", f"ps{pu}"], writes=[f"hT{fc}"])
            hk = [f"hT{fc}" for fc in range(NFC)]
            for jc in range(4):
                col = ex * 4 + jc
                yb = yc % 4
                yc += 1
                for dh in range(2):
                    pb = 4 + dh
                    for fc in range(NFC):
                        S.op("pe", lambda e, pb=pb, fc=fc, jc=jc, dh=dh, wdb=wdb: e.matmul(
                            C.ps[pb][:], lhsT=hT[:, fc, jc * 128:(jc + 1) * 128], rhs=wdt[wdb][:, fc, dh * 512:(dh + 1) * 512],
                            start=(fc == 0), stop=(fc == NFC - 1)),
                            reads=[hk[fc], f"wdt{wdb}_{fc // 11}"], writes=[f"ps{pb}"])
                    if dh == 0:
                        S.op("act", lambda e, pb=pb, yb=yb, col=col: e.mul(
                            out=yy[yb][:, 0:512], in_=C.ps[pb][:], mul=C.gsel[:, col:col + 1]),
                            reads=[f"ps{pb}"], writes=[f"yy{yb}_0"])
                    else:
                        S.op("dve", lambda e, pb=pb, yb=yb, col=col: e.tensor_scalar(
                            yy[yb][:, 512:1024], C.ps[pb][:], C.gsel[:, col:col + 1], None, ALU.mult),
                            reads=[f"ps{pb}"], writes=[f"yy{yb}_1"])
                pending_sc.append((yb, col))
            if ex == NE - 1:
                flush_scatters(len(pending_sc))
        S.flush()


def phase_final(C):
    nc, S, I = C.nc, C.S, C.I
    with ExitStack() as st:
        def sb(name, shape, dt):
            return st.enter_context(nc.sbuf_tensor(name, shape, dt))

        g32 = sb("fg32", [128, D], F32)
        xt = [sb(f"fxt{i}", [128, D], F32) for i in range(3)]
        ot = [sb(f"fot{i}", [128, D], F32) for i in range(3)]
        junk = sb("fjunk", [128, D], F32)
        ss = [sb(f"fss{i}", [128, 1], F32) for i in range(3)]
        rstd = [sb(f"frstd{i}", [128, 1], F32) for i in range(3)]
        S.op("sp", lambda e: e.dma_start(out=g32[:], in_=bcast_rows(I["final_norm_g"], 128)), writes=["fg32"], dma=True)
        S.op("dve", lambda e: e.tensor_scalar(g32[:], g32[:], 32.0, None, ALU.mult), reads=["fg32"], writes=["fg32"])
        for i in range(NT):
            b = i % 3
            S.op("sp", lambda e, i=i, b=b: e.dma_start(out=xt[b][:], in_=C.X2[i * 128:(i + 1) * 128, :]),
                 writes=[f"fxt{b}"], dma=True)
            S.op("dve", lambda e, b=b: e.memset(ss[b][:], 0.0), writes=[f"fss{b}"])
            S.op("act", lambda e, b=b: e.activation(out=junk[:], in_=xt[b][:], func=AF.Square, accum_out=ss[b][:]),
                 reads=[f"fxt{b}", f"fss{b}"], writes=[f"fss{b}", "fjunk"])
            S.op("act", lambda e, b=b: e.activation(out=ss[b][:], in_=ss[b][:], func=AF.Sqrt, bias=float(D * EPS),
                                                    scale=1.0), reads=[f"fss{b}"], writes=[f"fss{b}"])
            S.op("dve", lambda e, b=b: e.reciprocal(rstd[b][:], ss[b][:]), reads=[f"fss{b}"], writes=[f"frstd{b}"])
            S.op("dve", lambda e, b=b: e.scalar_tensor_tensor(out=ot[b][:], in0=xt[b][:], scalar=rstd[b][:, 0:1],
                                                              in1=g32[:], op0=ALU.mult, op1=ALU.mult),
                 reads=[f"fxt{b}", f"frstd{b}", "fg32"], writes=[f"fot{b}"])
            S.op("pool", lambda e, i=i, b=b: e.dma_start(out=C.out[i * 128:(i + 1) * 128, :], in_=ot[b][:]),
                 reads=[f"fot{b}"], writes=[f"out{i}"], dma=True)
        S.flush()
```

```python
import math
import os
from contextlib import ExitStack

import numpy as np
import concourse.bass as bass
import concourse.mybir as mybir
from concourse.bass_utils import run_bass_kernel_spmd

F32 = mybir.dt.float32
BF16 = mybir.dt.bfloat16
I32 = mybir.dt.int32
ALU = mybir.AluOpType
AF = mybir.ActivationFunctionType
AX = mybir.AxisListType

L = 4096
D = 1024
NT = L // 128
HW = 512
NE = 16
CAP = 512
FF = 2816
NFC = FF // 128
EPS = 1e-6
NFFT = 2 * L

SAME_ENG_SYNC = True


class Instr:
    __slots__ = ("eng", "fn", "deps", "dma", "signal", "sigidx", "lane", "val")

    def __init__(self, eng, fn, dma):
        self.eng = eng
        self.fn = fn
        self.dma = dma
        self.deps = {}
        self.signal = False
        self.sigidx = -1
        self.lane = None
        self.val = 0


class Sched:
    ENG = ("pe", "act", "dve", "pool", "sp")
    NL = 4
    ND = 12

    def __init__(self, nc, stack):
        self.nc = nc
        self.esem = {}
        for e in ("pe", "act", "dve", "pool"):
            self.esem[e] = [stack.enter_context(nc.semaphore(f"es_{e}{i}")) for i in range(self.NL)]
        self.dsem = {}
        for q in ("sp", "act", "pool"):
            self.dsem[q] = [stack.enter_context(nc.semaphore(f"ds_{q}{i}")) for i in range(self.ND)]
        self.phase_sem = stack.enter_context(nc.semaphore("phase"))
        self.phase = 0
        self.sigcount = {e: 0 for e in self.ENG}
        self.dmacount = {q: 0 for q in ("sp", "act", "pool")}
        self.pending = {e: [] for e in self.ENG}
        self.state = {}
        self.known = {e: {s: -1 for s in self.ENG} for e in self.ENG}
        self.known_dma = {e: {} for e in self.ENG}
        self.bar_tile = stack.enter_context(nc.sbuf_tensor("bar_tile", [128, 8], F32))
        self.n_instr = 0

    def op(self, eng, fn, reads=(), writes=(), dma=False):
        ins = Instr(eng, fn, dma)
        st = self.state
        for k in reads:
            s = st.get(k)
            if s is not None and s[0] is not None:
                ins.deps[id(s[0])] = (s[0], "raw")
        for k in writes:
            s = st.get(k)
            if s is not None:
                if s[0] is not None:
                    ins.deps[id(s[0])] = (s[0], "raw")
                for r in s[1].values():
                    if id(r) not in ins.deps:
                        ins.deps[id(r)] = (r, "war")
                for r in s[2]:
                    if id(r) not in ins.deps:
                        ins.deps[id(r)] = (r, "war")
        for k in reads:
            s = st.get(k)
            if s is None:
                s = [None, {}, []]
                st[k] = s
            if dma:
                s[2].append(ins)
            else:
                s[1][eng] = ins
        for k in writes:
            st[k] = [ins, {}, []]
        self.pending[eng].append(ins)
        self.n_instr += 1
        return ins

    @staticmethod
    def _needs_wait(ins, d, kind):
        if d.dma or ins.dma:
            return True
        if d.eng != ins.eng:
            return True
        if ins.eng == "pe":
            return False
        if kind == "war":
            return False
        return SAME_ENG_SYNC

    def flush(self):
        nc = self.nc
        pend = self.pending
        for e in self.ENG:
            for ins in pend[e]:
                for (d, kind) in ins.deps.values():
                    if not d.dma and self._needs_wait(ins, d, kind):
                        d.signal = True
        last_sig = {}
        for e in ("pe", "act", "dve", "pool"):
            comp = [i for i in pend[e] if not i.dma]
            if comp:
                comp[-1].signal = True
        for e in self.ENG:
            for ins in pend[e]:
                if ins.dma:
                    j = self.dmacount[e]
                    self.dmacount[e] += 1
                    ins.lane = j % self.ND
                    ins.val = 16 * (j // self.ND + 1)
                elif ins.signal:
                    ins.sigidx = self.sigcount[e]
                    self.sigcount[e] += 1
        for e in ("pe", "act", "dve", "pool"):
            last_sig[e] = self.sigcount[e] - 1
        phase = self.phase
        sched = self

        def wait_sig(eobj, me, src, sigidx):
            if sched.known[me][src] >= sigidx:
                return
            eobj.wait_ge(sched.esem[src][sigidx % sched.NL], sigidx // sched.NL + 1)
            sched.known[me][src] = sigidx

        def wait_dma(eobj, me, q, lane, val):
            kd = sched.known_dma[me]
            if kd.get((q, lane), 0) >= val:
                return
            eobj.wait_ge(sched.dsem[q][lane], val)
            kd[(q, lane)] = val

        def run(me, eobj):
            if phase > 0:
                eobj.wait_ge(sched.phase_sem, phase)
            for ins in pend[me]:
                for (d, kind) in ins.deps.values():
                    if not sched._needs_wait(ins, d, kind):
                        continue
                    if d.dma:
                        wait_dma(eobj, me, d.eng, d.lane, d.val)
                    else:
                        wait_sig(eobj, me, d.eng, d.sigidx)
                if ins.dma:
                    if ins.val > 16:
                        wait_dma(eobj, me, me, ins.lane, ins.val - 16)
                    r = ins.fn(eobj)
                    r.then_inc(sched.dsem[me][ins.lane], 16)
                else:
                    r = ins.fn(eobj)
                    if ins.signal:
                        r.then_inc(sched.esem[me][ins.sigidx % sched.NL], 1)
            if me == "pool":
                for src in ("pe", "act", "dve"):
                    if last_sig[src] >= 0:
                        wait_sig(eobj, me, src, last_sig[src])
                if last_sig["pool"] >= 0 and SAME_ENG_SYNC:
                    wait_sig(eobj, me, "pool", last_sig["pool"])
                for q in ("sp", "act", "pool"):
                    n = sched.dmacount[q]
                    for lane in range(sched.ND):
                        cnt = (n - lane + sched.ND - 1) // sched.ND if n > lane else 0
                        if cnt > 0:
                            wait_dma(eobj, me, q, lane, 16 * cnt)
                eobj.memset(sched.bar_tile[:], 0.0).then_inc(sched.phase_sem, 1)

        with nc.Block() as block:
            @block.tensor
            def _(e):
                run("pe", e)

            @block.scalar
            def _(e):
                run("act", e)

            @block.vector
            def _(e):
                run("dve", e)

            @block.gpsimd
            def _(e):
                run("pool", e)

            @block.sync
            def _(e):
                run("sp", e)

        self.phase += 1
        self.pending = {e: [] for e in self.ENG}
        self.state = {}

    def final_wait(self):
        nc = self.nc
        phase = self.phase
        sched = self
        with nc.Block() as block:
            @block.tensor
            def _(e):
                e.wait_ge(sched.phase_sem, phase)

            @block.scalar
            def _(e):
                e.wait_ge(sched.phase_sem, phase)

            @block.vector
            def _(e):
                e.wait_ge(sched.phase_sem, phase)

            @block.gpsimd
            def _(e):
                e.wait_ge(sched.phase_sem, phase)

            @block.sync
            def _(e):
                e.wait_ge(sched.phase_sem, phase)


def bcast_rows(ap, nparts):
    return ap.partition_broadcast(nparts)


class Ctx:
    pass


C_last = {}


def build_program(debug=None):
    nc = bass.Bass("TRN2", target_bir_lowering=False)
    C = Ctx()
    C.nc = nc
    C.debug = debug

    SHAPES = {
        "x": [L, D], "attn_norm_g": [1, D], "w_in": [D, 3072], "conv_w": [3, 1536], "conv_b": [1, 1536],
        "filt_w1": [33, 64], "filt_b1": [64, 1], "filt_w2": [64, 64], "filt_b2": [64, 1],
        "filt_w3": [64, 2048], "filt_freq": [64, 1], "hyena_skip": [2, 512], "hyena_norm_g": [1, 512],
        "lambda_q1": [1, 64], "lambda_k1": [1, 64], "lambda_q2": [1, 64], "lambda_k2": [1, 64],
        "subln_g": [128, 1], "w_out": [D, D], "ffn_norm_g": [1, D], "w_router": [D, NE],
        "w_gate": [NE, D, FF], "w_up": [NE, D, FF], "w_down": [NE, FF, D], "final_norm_g": [1, D],
    }

    class LazyIn(dict):
        def __missing__(self, name):
            ap = nc.dram_tensor(name, list(SHAPES[name]), F32, kind="ExternalInput").ap()
            self[name] = ap
            return ap

    I = LazyIn()
    C.SHAPES = SHAPES
    C.I = I
    C.out = nc.dram_tensor("out", [L, D], F32, kind="ExternalOutput").ap()

    def dscratch(name, shape, dt):
        if debug:
            return nc.dram_tensor(name, list(shape), dt, kind="ExternalOutput").ap()
        return nc.dram_tensor(name, list(shape), dt).ap()

    C.dscratch = dscratch
    with ExitStack() as stack:
        S = Sched(nc, stack)
        C.S = S
        C.stack = stack
        C.ps = [stack.enter_context(nc.psum_tensor(f"ps{i}", [128, 512], F32)) for i in range(8)]
        C.ident = stack.enter_context(nc.sbuf_tensor("ident", [128, 128], F32))
        C.identb = stack.enter_context(nc.sbuf_tensor("identb", [128, 128], BF16))
        C.ones_f = stack.enter_context(nc.sbuf_tensor("ones_f", [128, 128], F32))
        C.ones_b = stack.enter_context(nc.sbuf_tensor("ones_b", [128, 128], BF16))
        S.op("pool", lambda e: e.memset(C.ones_f[:], 1.0), writes=["ones_f"])
        S.op("pool", lambda e: e.memset(C.ones_b[:], 1.0), writes=["ones_b"])
        C.tmp_id = stack.enter_context(nc.sbuf_tensor("tmp_id", [128, 128], F32))
        S.op("pool", lambda e: e.iota(C.tmp_id[:], [[1, 128]], base=0, channel_multiplier=-1,
                                      allow_small_or_imprecise_dtypes=True), writes=["tmp_id"])
        S.op("dve", lambda e: e.tensor_scalar(C.ident[:], C.tmp_id[:], 0.0, None, ALU.is_equal),
             reads=["tmp_id"], writes=["ident"])
        S.op("dve", lambda e: e.tensor_copy(C.identb[:], C.ident[:]), reads=["ident"], writes=["identb"])
        S.flush()

        C.MIXT = C.dscratch("MIXT", [8, 128, L], BF16)
        stages = [("proj", "phase_proj"), ("attn", "phase_attn"), ("dft", "phase_dftgen"), ("filt", "phase_filter"),
                  ("conv", "phase_conv"), ("oproj", "phase_oproj"), ("topk", "phase_topk"), ("moe", "phase_moe"),
                  ("final", "phase_final")]
        only = os.environ.get("MK_ONLY")
        for name, fname in stages:
            if only and name not in only.split(","):
                continue
            globals()[fname](C)
            if debug == name:
                break
        S.final_wait()
    C_last["I"] = I
    C_last["SHAPES"] = SHAPES
    return nc


def phase_proj(C):
    nc, S, I = C.nc, C.S, C.I
    C.U = [C.dscratch(f"U{g}", [L, HW], F32) for g in range(3)]
    C.QT = C.dscratch("QT", [4, 128, L], BF16)
    C.KT = C.dscratch("KT", [4, 128, L], BF16)
    C.VA = C.dscratch("VA", [L, HW], BF16)
    C.VH = C.dscratch("VH", [L, HW], BF16)
    with ExitStack() as st:
        def sb(name, shape, dt):
            return st.enter_context(nc.sbuf_tensor(name, shape, dt))

        hnT = sb("hnT", [128, 8, L + 2], BF16)
        g32 = sb("g32", [128, D], F32)
        xt = [sb(f"xt{i}", [128, D], F32) for i in range(2)]
        hn = [sb(f"hn{i}", [128, D], F32) for i in range(2)]
        junk = sb("junk", [128, D], F32)
        ss = [sb(f"ss{i}", [128, 1], F32) for i in range(2)]
        rstd = [sb(f"rstd{i}", [128, 1], F32) for i in range(2)]

        S.op("sp", lambda e: e.dma_start(out=g32[:], in_=bcast_rows(I["attn_norm_g"], 128)),
             writes=["g32"], dma=True)
        S.op("dve", lambda e: e.tensor_scalar(g32[:], g32[:], 32.0, None, ALU.mult),
             reads=["g32"], writes=["g32"])
        S.op("pool", lambda e: e.memset(hnT[:, :, 0:1], 0.0), writes=["hnT_pad0"])
        S.op("pool", lambda e: e.memset(hnT[:, :, L + 1:L + 2], 0.0), writes=["hnT_pad1"])

        for i in range(NT):
            b = i % 2
            S.op("sp", lambda e, i=i, b=b: e.dma_start(out=xt[b][:], in_=I["x"][i * 128:(i + 1) * 128, :]),
                 writes=[f"xt{b}"], dma=True)
            S.op("dve", lambda e, b=b: e.memset(ss[b][:], 0.0), writes=[f"ss{b}"])
            S.op("act", lambda e, b=b: e.activation(out=junk[:], in_=xt[b][:], func=AF.Square,
                                                    accum_out=ss[b][:]),
                 reads=[f"xt{b}", f"ss{b}"], writes=[f"ss{b}", "junk"])
            S.op("act", lambda e, b=b: e.activation(out=ss[b][:], in_=ss[b][:], func=AF.Sqrt,
                                                    bias=float(D * EPS), scale=1.0),
                 reads=[f"ss{b}"], writes=[f"ss{b}"])
            S.op("dve", lambda e, b=b: e.reciprocal(rstd[b][:], ss[b][:]),
                 reads=[f"ss{b}"], writes=[f"rstd{b}"])
            S.op("dve", lambda e, b=b: e.scalar_tensor_tensor(out=hn[b][:], in0=xt[b][:], scalar=rstd[b][:, 0:1],
                                                              in1=g32[:], op0=ALU.mult, op1=ALU.mult),
                 reads=[f"xt{b}", f"rstd{b}", "g32"], writes=[f"hn{b}"])
            for half in range(2):
                pb = (2 * i + half) % 2
                for kk in range(4):
                    k = half * 4 + kk
                    S.op("pe", lambda e, b=b, k=k, kk=kk, pb=pb: e.transpose(
                        out=C.ps[pb][:, kk * 128:(kk + 1) * 128], in_=hn[b][:, k * 128:(k + 1) * 128],
                        identity=C.ident[:]),
                        reads=[f"hn{b}", "ident"], writes=[f"ps{pb}"])
                eng = "act" if half == 0 else "dve"
                if eng == "act":
                    S.op("act", lambda e, i=i, half=half, pb=pb: e.copy(
                        out=hnT[:, half * 4:(half + 1) * 4, 1 + i * 128:1 + (i + 1) * 128],
                        in_=C.ps[pb][:].rearrange("p (k t) -> p k t", k=4)),
                        reads=[f"ps{pb}"], writes=[f"hnT_{i}_{half}"])
                else:
                    S.op("dve", lambda e, i=i, half=half, pb=pb: e.tensor_copy(
                        out=hnT[:, half * 4:(half + 1) * 4, 1 + i * 128:1 + (i + 1) * 128],
                        in_=C.ps[pb][:].rearrange("p (k t) -> p k t", k=4)),
                        reads=[f"ps{pb}"], writes=[f"hnT_{i}_{half}"])
        hnT_keys = [f"hnT_{i}_{h}" for i in range(NT) for h in range(2)] + ["hnT_pad0", "hnT_pad1"]

        wst = [sb(f"wst{i}", [128, 8, 512], F32) for i in range(2)]
        wbf = [sb(f"wbf{i}", [128, 8, 3, 512], BF16) for i in range(2)]
        cw = sb("cw", [128, 3, 1536], F32)
        cb = sb("cb", [128, 1536], F32)
        for j in range(3):
            S.op("sp", lambda e, j=j: e.dma_start(out=cw[:, j, :], in_=bcast_rows(I["conv_w"][j:j + 1, :], 128)),
                 writes=[f"cw{j}"], dma=True)
        S.op("sp", lambda e: e.dma_start(out=cb[:], in_=bcast_rows(I["conv_b"], 128)), writes=["cb"], dma=True)
        ost = [sb(f"ost{i}", [128, 512], F32) for i in range(3)]
        obf = [sb(f"obf{i}", [128, 512], BF16) for i in range(3)]
        oT = [sb(f"oT{i}", [128, 512], BF16) for i in range(3)]
        w_view = I["w_in"].rearrange("(k p) n -> p k n", p=128)
        psb = [2, 3, 4, 5]
        pcount = [0]
        ocount = [0]
        scale_q = 64 ** -0.5
        for g in range(6):
            wb = g % 2
            S.op("sp", lambda e, g=g, wb=wb: e.dma_start(out=wst[wb][:], in_=w_view[:, :, g * 512:(g + 1) * 512]),
                 writes=[f"wst{wb}"], dma=True)
            if g < 3:
                for j in range(3):
                    eng = "dve"
                    S.op(eng, lambda e, g=g, wb=wb, j=j: e.tensor_tensor(
                        out=wbf[wb][:, :, j, :], in0=wst[wb][:],
                        in1=cw[:, j, g * 512:(g + 1) * 512].unsqueeze(1).to_broadcast([128, 8, 512]),
                        op=ALU.mult),
                        reads=[f"wst{wb}", f"cw{j}"], writes=[f"wbf{wb}_{j}"])
                wkeys = [f"wbf{wb}_{j}" for j in range(3)]
            else:
                S.op("act", lambda e, wb=wb: e.copy(out=wbf[wb][:, :, 0, :], in_=wst[wb][:]),
                     reads=[f"wst{wb}"], writes=[f"wbf{wb}_0"])
                wkeys = [f"wbf{wb}_0"]
            if g in (0, 1, 2, 5):
                ntap = 3 if g < 3 else 1
                for i in range(NT):
                    pb = psb[pcount[0] % 4]
                    pcount[0] += 1
                    n_mm = ntap * 8
                    c = 0
                    for j in range(ntap):
                        for k in range(8):
                            off = i * 128 + j if g < 3 else i * 128 + 1
                            S.op("pe", lambda e, pb=pb, k=k, j=j, off=off, wb=wb, c=c, n_mm=n_mm: e.matmul(
                                C.ps[pb][:], lhsT=hnT[:, k, off:off + 128], rhs=wbf[wb][:, k, j, :],
                                start=(c == 0), stop=(c == n_mm - 1)),
                                reads=hnT_keys_for(i) + wkeys, writes=[f"ps{pb}"])
                            c += 1
                    ob = ocount[0] % 3
                    ocount[0] += 1
                    if g < 3:
                        S.op("dve", lambda e, pb=pb, ob=ob, g=g: e.tensor_tensor(
                            out=ost[ob][:], in0=C.ps[pb][:], in1=cb[:, g * 512:(g + 1) * 512], op=ALU.add),
                            reads=[f"ps{pb}", "cb"], writes=[f"ost{ob}"])
                        S.op("pool", lambda e, ob=ob, g=g, i=i: e.dma_start(
                            out=C.U[g][i * 128:(i + 1) * 128, :], in_=ost[ob][:]),
                            reads=[f"ost{ob}"], writes=[f"U{g}_{i}"], dma=True)
                        if g == 2:
                            S.op("act", lambda e, ob=ob: e.copy(out=obf[ob][:], in_=ost[ob][:]),
                                 reads=[f"ost{ob}"], writes=[f"obf{ob}"])
                            S.op("pool", lambda e, ob=ob, i=i: e.dma_start(
                                out=C.VH[i * 128:(i + 1) * 128, :], in_=obf[ob][:]),
                                reads=[f"obf{ob}"], writes=[f"VH_{i}"], dma=True)
                    else:
                        S.op("act", lambda e, pb=pb, ob=ob: e.copy(out=obf[ob][:], in_=C.ps[pb][:]),
                             reads=[f"ps{pb}"], writes=[f"obf{ob}"])
                        S.op("pool", lambda e, ob=ob, i=i: e.dma_start(
                            out=C.VA[i * 128:(i + 1) * 128, :], in_=obf[ob][:]),
                            reads=[f"obf{ob}"], writes=[f"VA_{i}"], dma=True)
            else:
                dst = C.QT if g == 3 else C.KT
                for h in range(4):
                    for tb in range(8):
                        pb = psb[pcount[0] % 4]
                        pcount[0] += 1
                        for k in range(8):
                            S.op("pe", lambda e, pb=pb, k=k, h=h, tb=tb, wb=wb: e.matmul(
                                C.ps[pb][:], lhsT=wbf[wb][:, k, 0, h * 128:(h + 1) * 128],
                                rhs=hnT[:, k, 1 + tb * 512:1 + (tb + 1) * 512],
                                start=(k == 0), stop=(k == 7)),
                                reads=hnT_keys_blk(tb) + wkeys, writes=[f"ps{pb}"])
                        ob = ocount[0] % 3
                        ocount[0] += 1
                        if g == 3:
                            S.op("act", lambda e, pb=pb, ob=ob: e.mul(out=oT[ob][:], in_=C.ps[pb][:], mul=scale_q),
                                 reads=[f"ps{pb}"], writes=[f"oT{ob}"])
                        else:
                            S.op("dve", lambda e, pb=pb, ob=ob: e.tensor_copy(out=oT[ob][:], in_=C.ps[pb][:]),
                                 reads=[f"ps{pb}"], writes=[f"oT{ob}"])
                        S.op("pool", lambda e, ob=ob, h=h, tb=tb, dst=dst: e.dma_start(
                            out=dst[h, :, tb * 512:(tb + 1) * 512], in_=oT[ob][:]),
                            reads=[f"oT{ob}"], writes=[f"{'QT' if dst is C.QT else 'KT'}_{h}_{tb}"], dma=True)
        S.flush()


def hnT_keys_for(i):
    ks = [f"hnT_{i}_0", f"hnT_{i}_1"]
    if i > 0:
        ks += [f"hnT_{i - 1}_0", f"hnT_{i - 1}_1"]
    else:
        ks += ["hnT_pad0"]
    if i < NT - 1:
        ks += [f"hnT_{i + 1}_0", f"hnT_{i + 1}_1"]
    else:
        ks += ["hnT_pad1"]
    return ks


def hnT_keys_blk(tb):
    ks = []
    for i in range(tb * 4, tb * 4 + 4):
        ks += [f"hnT_{i}_0", f"hnT_{i}_1"]
    return ks


C_last = {}


def kernel(**inputs):
    debug = os.environ.get("MK_DEBUG")
    x = np.asarray(inputs["x"], dtype=np.float32)
    nc = build_program(debug)
    common = {}
    for name in C_last["I"].keys():
        if name == "x":
            continue
        common[name] = np.ascontiguousarray(
            np.asarray(inputs[name], dtype=np.float32).reshape(C_last["SHAPES"][name]))
    in_maps = []
    for c in range(8):
        m = dict(common)
        m["x"] = np.ascontiguousarray(x[c])
        in_maps.append(m)
    res = run_bass_kernel_spmd(nc, in_maps, core_ids=list(range(8)))
    if debug:
        return res
    out = np.stack([np.asarray(r["out"], dtype=np.float32) for r in res.results], axis=0)
    return out


def phase_attn(C):
    nc, S, I = C.nc, C.S, C.I
    if not hasattr(C, "QT"):
        C.QT = C.dscratch("QT", [4, 128, L], BF16)
        C.KT = C.dscratch("KT", [4, 128, L], BF16)
        C.VA = C.dscratch("VA", [L, HW], BF16)
    XTAB = C.dscratch("XTAB", [4, 3, 4, L], BF16)
    KR = 68
    with ExitStack() as st0:
        def sb0(name, shape, dt):
            return st0.enter_context(nc.sbuf_tensor(name, shape, dt))
        PJ = L // 128
        pos = sb0("apos", [128, PJ], F32)
        posi = sb0("aposi", [128, PJ], I32)
        plo = sb0("aplo", [128, PJ], F32)
        phi = sb0("aphi", [128, PJ], F32)
        NR = 2 + 4 * 4
        rowt = sb0("arows", [128, NR, PJ], BF16)
        S.op("pool", lambda e: e.iota(pos[:], [[1, PJ]], base=0, channel_multiplier=PJ,
                                      allow_small_or_imprecise_dtypes=True), writes=["apos"])
        S.op("pool", lambda e: e.iota(posi[:], [[1, PJ]], base=0, channel_multiplier=PJ), writes=["aposi"])
        S.op("dve", lambda e: e.tensor_scalar(posi[:], posi[:], 127, None, ALU.bitwise_and), reads=["aposi"], writes=["aposi"])
        S.op("dve", lambda e: e.tensor_copy(plo[:], posi[:]), reads=["aposi"], writes=["aplo"])
        S.op("dve", lambda e: e.tensor_tensor(out=phi[:], in0=pos[:], in1=plo[:], op=ALU.subtract),
             reads=["apos", "aplo"], writes=["aphi"])
        S.op("pool", lambda e: e.memset(rowt[:, 0, :], 1.0), writes=["arow_one"])
        S.op("pool", lambda e: e.memset(rowt[:, 1, :], -1.0), writes=["arow_none"])
        ridx = {"one": 0, "none": 1}
        for h in range(4):
            slope = 2.0 ** (-8.0 * (h + 1) / 4)
            for k_, (nm, src, sk, mul) in enumerate((("a", phi, "aphi", slope), ("b", plo, "aplo", slope),
                                                     ("na", phi, "aphi", -slope), ("nb", plo, "aplo", -slope))):
                ri = 2 + 4 * h + k_
                ridx[(h, nm)] = ri
                S.op("dve", lambda e, ri=ri, src=src, mul=mul: e.tensor_scalar(rowt[:, ri, :], src[:], float(mul), None, ALU.mult),
                     reads=[sk], writes=[f"arow{ri}"])
            layout = (("one", "one", "na", "nb"), ("a", "b", "one", "one"), ("na", "nb", "none", "none"))
            for v, names in enumerate(layout):
                for rr, nm in enumerate(names):
                    ri = ridx[nm] if nm in ("one", "none") else ridx[(h, nm)]
                    key = f"arow_{nm}" if nm in ("one", "none") else f"arow{ri}"
                    S.op("sp", lambda e, h=h, v=v, rr=rr, ri=ri: e.dma_start(
                        out=XTAB[h, v, rr, :].rearrange("(p j) -> p j", j=PJ), in_=rowt[:, ri, :]),
                        reads=[key], writes=[f"XTAB{h}_{v}_{rr}"], dma=True)
        S.flush()
    with ExitStack() as st:
        def sb(name, shape, dt):
            return st.enter_context(nc.sbuf_tensor(name, shape, dt))

        qa = [[sb(f"qa{i}_{c}", [KR, L], BF16) for c in range(2)] for i in range(2)]
        ka = [[[sb(f"ka{i}_{c}_{v}", [KR, L], BF16) for v in range(2)] for c in range(2)] for i in range(2)]
        vall = sb("vall", [128, NT, HW], BF16)
        D0 = sb("D0", [128, 512], F32)
        Dr = [sb(f"Dr{r}", [128, 512], F32) for r in range(4)]
        lamv = [sb(f"lamv{i}", [128, 64], F32) for i in range(4)]
        lt = [sb(f"lt{i}", [128, 1], F32) for i in range(4)]
        neg_lam = sb("neg_lam", [128, 1], F32)
        gsc = sb("gsc", [128, 1], F32)
        stt = [sb(f"stt{i}", [128, 512], F32) for i in range(4)]
        ptt = [sb(f"ptt{i}", [128, 512], BF16) for i in range(4)]
        r0 = sb("r0", [128, 512], F32)
        t0 = sb("t0", [128, 512], F32)
        r1 = sb("r1", [128, 512], F32)
        t1 = sb("t1", [128, 512], F32)
        oo = sb("oo", [128, 512], F32)
        sq = sb("sq", [128, 512], F32)
        rs = sb("rs", [128, 512], F32)
        res = [sb(f"res{i}", [128, 512], BF16) for i in range(2)]
        S.op("sp", lambda e: e.dma_start(out=vall[:], in_=C.VA.rearrange("(i p) n -> p i n", p=128)),
             writes=["vall"], dma=True)
        S.op("pool", lambda e: e.iota(D0[:], [[1, 512]], base=0, channel_multiplier=-1,
                                      allow_small_or_imprecise_dtypes=True), writes=["D0"])
        for r in range(4):
            S.op("dve", lambda e, r=r: e.tensor_scalar(Dr[r][:], D0[:], -1.0, float(128 * r), ALU.mult, ALU.add),
                 reads=["D0"], writes=[f"Dr{r}"])
            S.op("dve", lambda e, r=r: e.tensor_scalar(Dr[r][:], Dr[r][:], 0.0, 2.0, ALU.max, ALU.mult),
                 reads=[f"Dr{r}"], writes=[f"Dr{r}"])
        for i, nm in enumerate(["lambda_q1", "lambda_k1", "lambda_q2", "lambda_k2"]):
            S.op("sp", lambda e, i=i, nm=nm: e.dma_start(out=lamv[i][:], in_=bcast_rows(I[nm], 128)),
                 writes=[f"lamv{i}"], dma=True)
        S.op("sp", lambda e: e.dma_start(out=gsc[:], in_=I["subln_g"]), writes=["gsc"], dma=True)
        lam_init = 0.8 - 0.6 * math.exp(0.0)
        S.op("dve", lambda e: e.tensor_scalar(gsc[:], gsc[:], float(1.0 - lam_init), None, ALU.mult),
             reads=["gsc"], writes=["gsc"])
        for pi in range(2):
            S.op("dve", lambda e, pi=pi: e.tensor_tensor(out=lamv[2 * pi][:], in0=lamv[2 * pi][:],
                                                         in1=lamv[2 * pi + 1][:], op=ALU.mult),
                 reads=[f"lamv{2 * pi}", f"lamv{2 * pi + 1}"], writes=[f"lamv{2 * pi}"])
            S.op("dve", lambda e, pi=pi: e.reduce_sum(out=lt[pi][:], in_=lamv[2 * pi][:], axis=AX.X),
                 reads=[f"lamv{2 * pi}"], writes=[f"lt{pi}"])
            S.op("act", lambda e, pi=pi: e.activation(out=lt[2 + pi][:], in_=lt[pi][:], func=AF.Exp),
                 reads=[f"lt{pi}"], writes=[f"lt{2 + pi}"])
        S.op("dve", lambda e: e.tensor_tensor(out=neg_lam[:], in0=lt[3][:], in1=lt[2][:], op=ALU.subtract),
             reads=["lt2", "lt3"], writes=["neg_lam"])
        S.op("dve", lambda e: e.tensor_scalar(neg_lam[:], neg_lam[:], float(-lam_init), None, ALU.add),
             reads=["neg_lam"], writes=["neg_lam"])

        units = [(h, b, a) for h in range(4) for b in range(8) for a in range(NT)]
        LAG = 1
        U = len(units)

        def load_head(h):
            hb = h % 2
            for c in range(2):
                S.op("sp", lambda e, c=c: e.dma_start(out=qa[hb][c][0:64, :], in_=C.QT[h, c * 64:(c + 1) * 64, :]),
                     writes=[f"qa{hb}_{c}"], dma=True)
                S.op("sp", lambda e, c=c: e.dma_start(out=qa[hb][c][64:68, :], in_=XTAB[h, 0]),
                     writes=[f"qa{hb}_{c}x"], dma=True)
                for v in range(2):
                    S.op("sp", lambda e, c=c, v=v: e.dma_start(out=ka[hb][c][v][0:64, :], in_=C.KT[h, c * 64:(c + 1) * 64, :]),
                         writes=[f"ka{hb}_{c}_{v}"], dma=True)
                    S.op("sp", lambda e, c=c, v=v: e.dma_start(out=ka[hb][c][v][64:68, :], in_=XTAB[h, 1 + v]),
                         writes=[f"ka{hb}_{c}_{v}x"], dma=True)

        def stage_a(u):
            h, b, a = units[u]
            slope = 2.0 ** (-8.0 * (h + 1) / 4)
            hb = h % 2
            if (h, b, a) == (0, 0, 0):
                load_head(0)
            if (b, a) == (2, 0) and h + 1 < 4:
                load_head(h + 1)
            v = 1 if a > 4 * b + 3 else 0
            diag = (4 * b <= a <= 4 * b + 3)
            for c in range(2):
                sbk = (2 * u + c) % 4
                psn = 4 + sbk
                S.op("pe", lambda e, c=c, psn=psn: e.matmul(
                    C.ps[psn][:], lhsT=ka[hb][c][v][:, a * 128:(a + 1) * 128],
                    rhs=qa[hb][c][:, b * 512:(b + 1) * 512], start=True, stop=True),
                    reads=[f"qa{hb}_{c}", f"qa{hb}_{c}x", f"ka{hb}_{c}_{v}", f"ka{hb}_{c}_{v}x"], writes=[f"ps{psn}"])
            for c in range(2):
                sbk = (2 * u + c) % 4
                psn = 4 + sbk
                if diag:
                    r = a - 4 * b
                    S.op("dve", lambda e, r=r, sbk=sbk, psn=psn: e.scalar_tensor_tensor(
                        out=stt[sbk][:], in0=Dr[r][:], scalar=float(-slope), in1=C.ps[psn][:], op0=ALU.mult, op1=ALU.add),
                        reads=[f"Dr{r}", f"ps{psn}"], writes=[f"stt{sbk}"])
                    S.op("act", lambda e, sbk=sbk: e.activation(out=ptt[sbk][:], in_=stt[sbk][:], func=AF.Exp),
                         reads=[f"stt{sbk}"], writes=[f"ptt{sbk}"])
                else:
                    S.op("act", lambda e, sbk=sbk, psn=psn: e.activation(out=ptt[sbk][:], in_=C.ps[psn][:], func=AF.Exp),
                         reads=[f"ps{psn}"], writes=[f"ptt{sbk}"])

        def stage_b(u):
            h, b, a = units[u]
            for c in range(2):
                sbk = (2 * u + c) % 4
                S.op("pe", lambda e, c=c, sbk=sbk: e.matmul(
                    C.ps[2 * c][:], lhsT=vall[:, a, h * 128:(h + 1) * 128], rhs=ptt[sbk][:],
                    start=(a == 0), stop=(a == NT - 1)),
                    reads=["vall", f"ptt{sbk}"], writes=[f"ps{2 * c}"])
            for c in range(2):
                sbk = (2 * u + c) % 4
                S.op("pe", lambda e, c=c, sbk=sbk: e.matmul(
                    C.ps[2 * c + 1][:], lhsT=C.ones_b[:], rhs=ptt[sbk][:],
                    start=(a == 0), stop=(a == NT - 1)),
                    reads=["ones_b", f"ptt{sbk}"], writes=[f"ps{2 * c + 1}"])
            if a != NT - 1:
                return
            S.op("dve", lambda e: e.reciprocal(r0[:], C.ps[1][:]), reads=["ps1"], writes=["r0"])
            S.op("dve", lambda e: e.tensor_tensor(out=t0[:], in0=C.ps[0][:], in1=r0[:], op=ALU.mult),
                 reads=["ps0", "r0"], writes=["t0"])
            S.op("dve", lambda e: e.reciprocal(r1[:], C.ps[3][:]), reads=["ps3"], writes=["r1"])
            S.op("dve", lambda e: e.tensor_tensor(out=t1[:], in0=C.ps[2][:], in1=r1[:], op=ALU.mult),
                 reads=["ps2", "r1"], writes=["t1"])
            S.op("dve", lambda e: e.scalar_tensor_tensor(out=oo[:], in0=t1[:], scalar=neg_lam[:, 0:1], in1=t0[:],
                                                         op0=ALU.mult, op1=ALU.add),
                 reads=["t1", "t0", "neg_lam"], writes=["oo"])
            S.op("dve", lambda e: e.tensor_tensor(out=sq[:], in0=oo[:], in1=oo[:], op=ALU.mult),
                 reads=["oo"], writes=["sq"])
            S.op("pe", lambda e: e.matmul(C.ps[1][:], lhsT=C.ones_f[:], rhs=sq[:], start=True, stop=True),
                 reads=["ones_f", "sq"], writes=["ps1"])
            S.op("act", lambda e: e.activation(out=rs[:], in_=C.ps[1][:], func=AF.Sqrt,
                                               bias=float(EPS), scale=1.0 / 128.0),
                 reads=["ps1"], writes=["rs"])
            S.op("dve", lambda e: e.reciprocal(rs[:], rs[:]), reads=["rs"], writes=["rs"])
            rb = (h * 8 + b) % 2
            S.op("dve", lambda e: e.scalar_tensor_tensor(out=res[rb][:], in0=oo[:], scalar=gsc[:, 0:1],
                                                         in1=rs[:], op0=ALU.mult, op1=ALU.mult),
                 reads=["oo", "gsc", "rs"], writes=[f"res{rb}"])
            S.op("sp", lambda e: e.dma_start(out=C.MIXT[4 + h, :, b * 512:(b + 1) * 512], in_=res[rb][:]),
                 reads=[f"res{rb}"], writes=[f"MIXT_{4 + h}_{b}"], dma=True)

        head0_keys = ["vall"] + [f"qa0_{c}" for c in range(2)] + [f"qa0_{c}x" for c in range(2)] + \
            [f"ka0_{c}_{v}" for c in range(2) for v in range(2)] + [f"ka0_{c}_{v}x" for c in range(2) for v in range(2)]
        def emit_precast():
            NPRE = 9
            C.NPRE = NPRE
            C.WGB = C.dscratch("WGB", [NPRE, D, FF], BF16)
            C.WUB = C.dscratch("WUB", [NPRE, D, FF], BF16)
            C.WDB = C.dscratch("WDB", [NPRE, FF, D], BF16)
            for ex in range(NPRE):
                for (src, dst, nm) in ((I["w_gate"], C.WGB, "g"), (I["w_up"], C.WUB, "u")):
                    sv = src[ex].rearrange("d (a f) -> (d a) f", a=2)
                    dv = dst[ex].rearrange("d (a f) -> (d a) f", a=2)
                    for q in range(4):
                        S.op("pool", lambda e, sv=sv, dv=dv, q=q: e.dma_start(out=dv[q * 512:(q + 1) * 512, :],
                                                                             in_=sv[q * 512:(q + 1) * 512, :]),
                             reads=head0_keys, writes=[f"W{nm}B{ex}_{q}"], dma=True)
                for q in range(4):
                    S.op("pool", lambda e, ex=ex, q=q: e.dma_start(out=C.WDB[ex, q * 704:(q + 1) * 704, :],
                                                                   in_=I["w_down"][ex, q * 704:(q + 1) * 704, :]),
                         reads=head0_keys, writes=[f"WdB{ex}_{q}"], dma=True)

        for idx in range(U + LAG):
            if idx < U:
                stage_a(idx)
            if idx == 0:
                emit_precast()
            if idx - LAG >= 0:
                stage_b(idx - LAG)
        S.flush()


def phase_dftgen(C):
    nc, S, I = C.nc, C.S, C.I
    C.MDc = C.dscratch("MDc", [16, 128, 32, 256], BF16)
    C.MDs = C.dscratch("MDs", [16, 128, 32, 256], BF16)
    N = NFFT
    with ExitStack() as st:
        def sb(name, shape, dt):
            return st.enter_context(nc.sbuf_tensor(name, shape, dt))

        brow = sb("brow", [128, L], F32)
        c2b1 = sb("c2b1", [128, L], F32)
        acol = sb("acol", [128, NT], F32)
        a2s = sb("a2s", [128, NT], F32)
        tA = [sb(f"tA{i}", [128, L], I32) for i in range(2)]
        tB = [sb(f"tB{i}", [128, L], F32) for i in range(2)]
        tC = [sb(f"tC{i}", [128, L], I32) for i in range(2)]
        tD = [sb(f"tD{i}", [128, L], F32) for i in range(2)]
        outs = [sb(f"mos{i}", [128, L], BF16) for i in range(2)]
        outc = [sb(f"moc{i}", [128, L], BF16) for i in range(2)]

        S.op("pool", lambda e: e.iota(brow[:], [[1, L]], base=0, channel_multiplier=0,
                                      allow_small_or_imprecise_dtypes=True), writes=["brow"])
        S.op("pool", lambda e: e.iota(c2b1[:], [[2, L]], base=1, channel_multiplier=0,
                                      allow_small_or_imprecise_dtypes=True), writes=["c2b1"])
        S.op("pool", lambda e: e.iota(acol[:], [[128, NT]], base=0, channel_multiplier=1,
                                      allow_small_or_imprecise_dtypes=True), writes=["acol"])
        S.op("dve", lambda e: e.tensor_scalar(a2s[:], acol[:], 2.0, float(2 * N), ALU.mult, ALU.add),
             reads=["acol"], writes=["a2s"])
        mdc_v = C.MDc.rearrange("c p r j -> p c r j")
        mds_v = C.MDs.rearrange("c p r j -> p c r j")
        for rc in range(NT):
            ob = rc % 2
            S.op("dve", lambda e, rc=rc, ob=ob: e.tensor_scalar(tA[ob][:], brow[:], acol[:, rc:rc + 1], None, ALU.mult),
                 reads=["brow", "acol"], writes=[f"tA{ob}"])
            S.op("dve", lambda e, ob=ob: e.tensor_scalar(tA[ob][:], tA[ob][:], N - 1, None, ALU.bitwise_and),
                 reads=[f"tA{ob}"], writes=[f"tA{ob}"])
            S.op("dve", lambda e, ob=ob: e.scalar_tensor_tensor(out=tB[ob][:], in0=tA[ob][:], scalar=4.0, in1=c2b1[:],
                                                                op0=ALU.mult, op1=ALU.add),
                 reads=[f"tA{ob}", "c2b1"], writes=[f"tB{ob}"])
            S.op("dve", lambda e, rc=rc, ob=ob: e.tensor_scalar(tC[ob][:], tB[ob][:], a2s[:, rc:rc + 1], None, ALU.add),
                 reads=[f"tB{ob}", "a2s"], writes=[f"tC{ob}"])
            S.op("dve", lambda e, ob=ob: e.tensor_scalar(tC[ob][:], tC[ob][:], 4 * N - 1, None, ALU.bitwise_and),
                 reads=[f"tC{ob}"], writes=[f"tC{ob}"])
            S.op("act", lambda e, ob=ob: e.activation(out=outs[ob][:], in_=tC[ob][:], func=AF.Sin,
                                                      bias=float(-math.pi), scale=float(math.pi / (2 * N))),
                 reads=[f"tC{ob}"], writes=[f"mos{ob}"])
            S.op("act", lambda e, ob=ob: e.activation(out=tD[ob][:], in_=tC[ob][:], func=AF.Abs,
                                                      bias=float(-math.pi), scale=float(math.pi / (2 * N))),
                 reads=[f"tC{ob}"], writes=[f"tD{ob}"])
            S.op("act", lambda e, ob=ob: e.activation(out=outc[ob][:], in_=tD[ob][:], func=AF.Sin,
                                                      bias=float(math.pi / 2), scale=-1.0),
                 reads=[f"tD{ob}"], writes=[f"moc{ob}"])
            S.op("sp", lambda e, rc=rc, ob=ob: e.dma_start(out=mds_v[:, :, rc, :],
                                                           in_=outs[ob][:].rearrange("p (c j) -> p c j", j=256)),
                 reads=[f"mos{ob}"], writes=[f"MDs_{rc}"], dma=True)
            S.op("sp", lambda e, rc=rc, ob=ob: e.dma_start(out=mdc_v[:, :, rc, :],
                                                           in_=outc[ob][:].rearrange("p (c j) -> p c j", j=256)),
                 reads=[f"moc{ob}"], writes=[f"MDc_{rc}"], dma=True)
        S.flush()


MAGIC = 12582912.0
TWO_PI = 2.0 * math.pi


def emit_sin(S, eng_v, out, x, tmpa, tmpb, keys_in, key_out, ktmp):
    S.op(eng_v, lambda e: e.tensor_scalar(tmpa, x, 1.0 / TWO_PI, MAGIC, ALU.mult, ALU.add),
         reads=keys_in, writes=[ktmp + "a"])
    S.op(eng_v, lambda e: e.tensor_scalar(tmpa, tmpa, MAGIC, -TWO_PI, ALU.subtract, ALU.mult),
         reads=[ktmp + "a"], writes=[ktmp + "a"])
    S.op(eng_v, lambda e: e.tensor_tensor(out=tmpb, in0=tmpa, in1=x, op=ALU.add),
         reads=[ktmp + "a"] + list(keys_in), writes=[ktmp + "b"])
    S.op(eng_v, lambda e: e.tensor_scalar(tmpb, tmpb, math.pi, -math.pi, ALU.min, ALU.max),
         reads=[ktmp + "b"], writes=[ktmp + "b"])
    S.op("act", lambda e: e.activation(out=out, in_=tmpb, func=AF.Sin), reads=[ktmp + "b"], writes=[key_out])


def phase_filter(C):
    nc, S, I = C.nc, C.S, C.I
    N = NFFT
    C.GD = C.dscratch("GD", [2, 2, NT, 128, HW], F32)
    with ExitStack() as st:
        def sb(name, shape, dt):
            return st.enter_context(nc.sbuf_tensor(name, shape, dt))

        hid2 = sb("hid2", [64, L + 128], F32)
        S.op("dve", lambda e: e.memset(hid2[:, L:L + 128], 0.0), writes=["hid2pad"])
        w3 = sb("fw3", [64, 2048], F32)
        st_outer = st
        st = ExitStack()
        st.__enter__()

        def sbi(name, shape, dt):
            return st.enter_context(nc.sbuf_tensor(name, shape, dt))
        sb_outer = sb
        sb = sbi
        prow = sb("prow", [64, 1], F32)
        prow_i = sb("prow_i", [64, 1], I32)
        band = sb("band", [64, 1], F32)
        phase = sb("phase", [64, 1], F32)
        dpos = sb("dpos", [64, L], F32)
        zT = sb("zT", [64, L], F32)
        ta = sb("fta", [64, L], F32)
        tb = sb("ftb", [64, L], F32)
        ang = sb("ang", [64, L], F32)
        S.op("pool", lambda e: e.iota(prow_i[:], [[0, 1]], base=-1, channel_multiplier=1), writes=["prow_i"])
        S.op("pool", lambda e: e.iota(prow[:], [[0, 1]], base=0, channel_multiplier=1,
                                      allow_small_or_imprecise_dtypes=True), writes=["prow"])
        S.op("dve", lambda e: e.tensor_scalar(prow_i[:], prow_i[:], 15, None, ALU.bitwise_and),
             reads=["prow_i"], writes=["prow_i"])
        b0 = 1e-4
        bstep = (15.0 - 1e-4) / 15.0
        S.op("dve", lambda e: e.tensor_scalar(band[:], prow_i[:], float(bstep), float(b0), ALU.mult, ALU.add),
             reads=["prow_i"], writes=["band"])
        S.op("dve", lambda e: e.tensor_scalar(band[:], band[:], float(TWO_PI / L), None, ALU.mult),
             reads=["band"], writes=["band"])
        S.op("dve", lambda e: e.tensor_scalar(phase[:], prow[:], 16.5, float(math.pi / 2), ALU.is_ge, ALU.mult),
             reads=["prow"], writes=["phase"])
        S.op("dve", lambda e: e.tensor_scalar(phase[:], phase[:], float(math.pi / 2), None, ALU.add),
             reads=["phase"], writes=["phase"])
        S.op("pool", lambda e: e.iota(dpos[:], [[1, L]], base=0, channel_multiplier=0,
                                      allow_small_or_imprecise_dtypes=True), writes=["dpos"])
        S.op("dve", lambda e: e.tensor_scalar(ang[:], dpos[:], band[:, 0:1], phase[:, 0:1], ALU.mult, ALU.add),
             reads=["dpos", "band", "phase"], writes=["ang"])
        emit_sin(S, "dve", zT[:], ang[:], ta[:], tb[:], ["ang"], "zT", "ft")
        S.op("dve", lambda e: e.tensor_scalar(zT[0:1, :], dpos[0:1, :], float(1.0 / (L - 1)), None, ALU.mult),
             reads=["dpos", "zT"], writes=["zT"])

        w1 = sb("fw1", [33, 64], F32)
        w2 = sb("fw2", [64, 64], F32)
        fr = sb("ffr", [64, 1], F32)
        fb1 = sb("fb1", [64, 1], F32)
        fb2 = sb("fb2", [64, 1], F32)
        for t_, nm in ((w1, "filt_w1"), (w2, "filt_w2"), (w3, "filt_w3"), (fr, "filt_freq"), (fb1, "filt_b1"),
                       (fb2, "filt_b2")):
            S.op("sp", lambda e, t_=t_, nm=nm: e.dma_start(out=t_[:], in_=I[nm]), writes=[nm], dma=True)
        S.op("dve", lambda e: e.tensor_tensor(out=fb1[:], in0=fb1[:], in1=fr[:], op=ALU.mult),
             reads=["filt_b1", "filt_freq"], writes=["filt_b1"])
        S.op("dve", lambda e: e.tensor_tensor(out=fb2[:], in0=fb2[:], in1=fr[:], op=ALU.mult),
             reads=["filt_b2", "filt_freq"], writes=["filt_b2"])
        hid1 = sb("hid1", [64, L], F32)
        pre = ang
        for (src, wt, wk, kk, bias, bk, dst, dk) in ((zT, w1, "filt_w1", 33, fb1, "filt_b1", hid1, "hid1"),
                                                     (hid1, w2, "filt_w2", 64, fb2, "filt_b2", hid2, "hid2")):
            srck = "zT" if src is zT else "hid1"
            for blk in range(8):
                pb = 4 + blk % 2
                S.op("pe", lambda e, pb=pb, wt=wt, kk=kk, src=src, blk=blk: e.matmul(
                    C.ps[pb][0:64, :], lhsT=wt[0:kk, :], rhs=src[0:kk, blk * 512:(blk + 1) * 512],
                    start=True, stop=True), reads=[wk, srck], writes=[f"ps{pb}"])
                S.op("dve", lambda e, pb=pb, blk=blk, bias=bias: e.tensor_scalar(
                    pre[:, blk * 512:(blk + 1) * 512], C.ps[pb][0:64, :], fr[:, 0:1], bias[:, 0:1], ALU.mult, ALU.add),
                    reads=[f"ps{pb}", "filt_freq", bk], writes=["fpre"])
            emit_sin(S, "dve", dst[:, 0:L], pre[:], ta[:], tb[:], ["fpre"], dk, "ft")

        S.flush()
        st.__exit__(None, None, None)
        st = st_outer
        sb = sb_outer
        delta = sb("delta", [128, HW], F32)
        negt = sb("negt", [128, NT], F32)
        min_decay = math.log(1e-2) / 1.5
        max_decay = math.log(1e-2) / 0.3
        dstep = (max_decay - min_decay) / (HW - 1)
        S.op("pool", lambda e: e.iota(delta[:], [[1, HW]], base=0, channel_multiplier=0,
                                      allow_small_or_imprecise_dtypes=True), writes=["delta"])
        S.op("dve", lambda e: e.tensor_scalar(delta[:], delta[:], float(-dstep), float(-min_decay), ALU.mult, ALU.add),
             reads=["delta"], writes=["delta"])
        S.op("pool", lambda e: e.iota(negt[:], [[128, NT]], base=0, channel_multiplier=1,
                                      allow_small_or_imprecise_dtypes=True), writes=["negt"])
        S.op("dve", lambda e: e.tensor_scalar(negt[:], negt[:], float(-1.0 / (L - 1)), None, ALU.mult),
             reads=["negt"], writes=["negt"])
        S.op("dve", lambda e: e.tensor_scalar(negts[:], negt[:], float(-1.0 / (L - 1)), None, ALU.add),
             reads=["negt"], writes=["negts"])
        psic = sb("psic", [128, NT], F32)
        psis = sb("psis", [128, NT], F32)
        npsic = sb("npsic", [128, NT], F32)
        fidx = sb("fidx", [128, NT], F32)
        S.op("pool", lambda e: e.iota(fidx[:], [[256, NT]], base=1, channel_multiplier=2,
                                      allow_small_or_imprecise_dtypes=True), writes=["fidx"])
        S.op("act", lambda e: e.activation(out=psis[:], in_=fidx[:], func=AF.Sin, scale=float(math.pi / (2 * N))),
             reads=["fidx"], writes=["psis"])
        S.op("act", lambda e: e.activation(out=psic[:], in_=fidx[:], func=AF.Sin, scale=float(-math.pi / (2 * N)),
                                           bias=float(math.pi / 2)),
             reads=["fidx"], writes=["psic"])
        S.op("dve", lambda e: e.tensor_scalar(npsic[:], psic[:], -1.0, None, ALU.mult), reads=["psic"], writes=["npsic"])

        hsum = sb("hsum", [128, NT, HW], BF16)
        hdif = sb("hdif", [128, NT, HW], BF16)
        win = [sb(f"win{i}", [128, HW], F32) for i in range(2)]
        wins = [sb(f"wins{i}", [128, HW], F32) for i in range(2)]
        negts = sb("negts", [128, NT], F32)
        fw = [sb(f"fwd{i}", [128, HW], F32) for i in range(2)]
        bw = [sb(f"bwd{i}", [128, HW], F32) for i in range(2)]
        af = [sb(f"absf{i}", [128, HW], F32) for i in range(2)]
        ab = [sb(f"absb{i}", [128, HW], F32) for i in range(2)]
        rn2 = sb("rn2", [128, HW], F32)
        nrn2 = sb("nrn2", [128, HW], F32)
        mtc = [sb(f"mtc{i}", [128, NT, 256], BF16) for i in range(2)]
        mts = [sb(f"mts{i}", [128, NT, 256], BF16) for i in range(2)]
        g1 = sb("g1", [128, HW], F32)
        gre = [sb(f"gre{i}", [128, HW], F32) for i in range(2)]
        gim = [sb(f"gim{i}", [128, HW], F32) for i in range(2)]
        for n in range(2):
            for dc in range(NT):
                b = dc % 2
                S.op("act", lambda e, b=b, dc=dc: e.activation(out=win[b][:], in_=delta[:], func=AF.Exp,
                                                               scale=negt[:, dc:dc + 1]),
                     reads=["delta", "negt"], writes=[f"win{b}"])
                S.op("act", lambda e, b=b, dc=dc: e.activation(out=wins[b][:], in_=delta[:], func=AF.Exp,
                                                               scale=negts[:, dc:dc + 1]),
                     reads=["delta", "negts"], writes=[f"wins{b}"])
                S.op("pe", lambda e, dc=dc, n=n: e.matmul(C.ps[0][:], lhsT=hid2[:, dc * 128:(dc + 1) * 128],
                                                          rhs=w3[:, n * 512:(n + 1) * 512], start=True, stop=True),
                     reads=["hid2", "filt_w3"], writes=["ps0"])
                S.op("pe", lambda e, dc=dc, n=n: e.matmul(C.ps[1][:], lhsT=hid2[:, dc * 128 + 1:(dc + 1) * 128 + 1],
                                                          rhs=w3[:, 1024 + n * 512:1024 + (n + 1) * 512],
                                                          start=True, stop=True),
                     reads=["hid2", "hid2pad", "filt_w3"], writes=["ps1"])
                S.op("dve", lambda e, b=b: e.tensor_tensor(out=fw[b][:], in0=C.ps[0][:], in1=win[b][:], op=ALU.mult),
                     reads=["ps0", f"win{b}"], writes=[f"fwd{b}"])
                S.op("dve", lambda e, b=b: e.tensor_tensor(out=bw[b][:], in0=C.ps[1][:], in1=wins[b][:], op=ALU.mult),
                     reads=["ps1", f"wins{b}"], writes=[f"bwd{b}"])
                S.op("act", lambda e, b=b: e.activation(out=af[b][:], in_=fw[b][:], func=AF.Abs),
                     reads=[f"fwd{b}"], writes=[f"absf{b}"])
                S.op("act", lambda e, b=b: e.activation(out=ab[b][:], in_=bw[b][:], func=AF.Abs),
                     reads=[f"bwd{b}"], writes=[f"absb{b}"])
                S.op("pe", lambda e, b=b, dc=dc: e.matmul(C.ps[2][:], lhsT=C.ones_f[:], rhs=af[b][:],
                                                          start=(dc == 0), stop=False),
                     reads=["ones_f", f"absf{b}"], writes=["ps2"])
                S.op("pe", lambda e, b=b, dc=dc: e.matmul(C.ps[2][:], lhsT=C.ones_f[:], rhs=ab[b][:],
                                                          start=False, stop=(dc == NT - 1)),
                     reads=["ones_f", f"absb{b}"], writes=["ps2"])
                S.op("pool", lambda e, b=b, dc=dc: e.tensor_tensor(out=hsum[:, dc, :], in0=fw[b][:], in1=bw[b][:],
                                                                   op=ALU.add),
                     reads=[f"fwd{b}", f"bwd{b}"], writes=[f"hsum{dc}"])
                S.op("pool", lambda e, b=b, dc=dc: e.tensor_tensor(out=hdif[:, dc, :], in0=fw[b][:], in1=bw[b][:],
                                                                   op=ALU.subtract),
                     reads=[f"fwd{b}", f"bwd{b}"], writes=[f"hdif{dc}"])
            S.op("dve", lambda e: e.reciprocal(rn2[:], C.ps[2][:]), reads=["ps2"], writes=["rn2"])
            S.op("dve", lambda e: e.tensor_scalar(rn2[:], rn2[:], float(2.0 / N), None, ALU.mult),
                 reads=["rn2"], writes=["rn2"])
            S.op("dve", lambda e: e.tensor_scalar(nrn2[:], rn2[:], -1.0, None, ALU.mult),
                 reads=["rn2"], writes=["nrn2"])
            hs_keys = [f"hsum{dc}" for dc in range(NT)]
            hd_keys = [f"hdif{dc}" for dc in range(NT)]
            def fload(cc2):
                mb = cc2 % 2
                S.op("sp", lambda e: e.dma_start(out=mtc[mb][:], in_=C.MDc[cc2]), writes=[f"mtc{mb}"], dma=True)
                S.op("sp", lambda e: e.dma_start(out=mts[mb][:], in_=C.MDs[cc2]), writes=[f"mts{mb}"], dma=True)
            fload(0)
            for cc2 in range(16):
                mb = cc2 % 2
                if cc2 + 1 < 16:
                    fload(cc2 + 1)
                for sub in range(2):
                    fc = cc2 * 2 + sub
                    gb = fc % 2
                    pc, ps_ = (4, 5) if gb == 0 else (6, 7)
                    for (pb, mt, mk, hh, hk) in ((pc, mtc, "mtc", hsum, hs_keys), (ps_, mts, "mts", hdif, hd_keys)):
                        for rc in range(NT):
                            S.op("pe", lambda e, pb=pb, mt=mt, mb=mb, rc=rc, sub=sub, hh=hh: e.matmul(
                                C.ps[pb][:], lhsT=mt[mb][:, rc, sub * 128:(sub + 1) * 128], rhs=hh[:, rc, :],
                                start=(rc == 0), stop=(rc == NT - 1)),
                                reads=[f"{mk}{mb}", hk[rc]], writes=[f"ps{pb}"])
                    S.op("dve", lambda e, fc=fc, ps_=ps_: e.tensor_scalar(g1[:], C.ps[ps_][:], psis[:, fc:fc + 1], None, ALU.mult),
                         reads=[f"ps{ps_}", "psis"], writes=["g1"])
                    S.op("dve", lambda e, fc=fc, gb=gb, pc=pc: e.scalar_tensor_tensor(
                        out=gre[gb][:], in0=C.ps[pc][:], scalar=psic[:, fc:fc + 1], in1=g1[:], op0=ALU.mult, op1=ALU.add),
                        reads=[f"ps{pc}", "psic", "g1"], writes=[f"gre{gb}"])
                    S.op("dve", lambda e, gb=gb: e.tensor_tensor(out=gre[gb][:], in0=gre[gb][:], in1=rn2[:], op=ALU.mult),
                         reads=[f"gre{gb}", "rn2"], writes=[f"gre{gb}"])
                    S.op("dve", lambda e, fc=fc, ps_=ps_: e.tensor_scalar(g1[:], C.ps[ps_][:], npsic[:, fc:fc + 1], None, ALU.mult),
                         reads=[f"ps{ps_}", "npsic"], writes=["g1"])
                    S.op("dve", lambda e, fc=fc, gb=gb, pc=pc: e.scalar_tensor_tensor(
                        out=gim[gb][:], in0=C.ps[pc][:], scalar=psis[:, fc:fc + 1], in1=g1[:], op0=ALU.mult, op1=ALU.add),
                        reads=[f"ps{pc}", "psis", "g1"], writes=[f"gim{gb}"])
                    S.op("dve", lambda e, gb=gb: e.tensor_tensor(out=gim[gb][:], in0=gim[gb][:], in1=rn2[:], op=ALU.mult),
                         reads=[f"gim{gb}", "rn2"], writes=[f"gim{gb}"])
                    S.op("act", lambda e, n=n, fc=fc, gb=gb: e.dma_start(out=C.GD[n, 0, fc], in_=gre[gb][:]),
                         reads=[f"gre{gb}"], writes=[f"GD{n}0{fc}"], dma=True)
                    S.op("act", lambda e, n=n, fc=fc, gb=gb: e.dma_start(out=C.GD[n, 1, fc], in_=gim[gb][:]),
                         reads=[f"gim{gb}"], writes=[f"GD{n}1{fc}"], dma=True)
        S.flush()


def phase_conv(C):
    nc, S, I = C.nc, C.S, C.I
    C.Z1 = C.dscratch("Z1", [L, HW], F32)
    with ExitStack() as st:
        def sb(name, shape, dt):
            return st.enter_context(nc.sbuf_tensor(name, shape, dt))

        zin = sb("zin", [128, NT, HW], BF16)
        wre = sb("wre", [128, NT, HW], BF16)
        wim = sb("wim", [128, NT, HW], BF16)
        mtc = [sb(f"cmtc{i}", [128, NT, 256], BF16) for i in range(2)]
        mts = [sb(f"cmts{i}", [128, NT, 256], BF16) for i in range(2)]
        gre = [sb(f"cgre{i}", [128, HW], F32) for i in range(2)]
        gim = [sb(f"cgim{i}", [128, HW], F32) for i in range(2)]
        xc0 = sb("xc0", [128, HW], F32)
        xs0 = sb("xs0", [128, HW], F32)
        xc = [xc0, xc0]
        xs = [xs0, xs0]
        ta = sb("cta", [128, HW], F32)
        tb = sb("ctb", [128, HW], F32)
        tc_ = sb("ctc", [128, HW], F32)
        td = sb("ctd", [128, HW], F32)
        gate = [sb(f"gate{i}", [128, HW], F32) for i in range(2)]
        zf = [sb(f"zf{i}", [128, HW], F32) for i in range(2)]
        zo = [sb(f"zo{i}", [128, HW], F32) for i in range(2)]
        skipb = sb("skipb", [128, 2, HW], F32)
        ngb = sb("ngb", [128, HW], F32)
        sq = ta
        gs = sb("cgs", [128, 8], F32)
        zn = tb
        mxt = [sb(f"mxt{i}", [128, 4, 128], BF16) for i in range(2)]

        S.op("sp", lambda e: e.dma_start(out=zin[:], in_=C.VH.rearrange("(i p) n -> p i n", p=128)),
             writes=["zin"], dma=True)
        for n in range(2):
            S.op("sp", lambda e, n=n: e.dma_start(out=skipb[:, n, :], in_=bcast_rows(I["hyena_skip"][n:n + 1, :], 128)),
                 writes=[f"skipb{n}"], dma=True)
        S.op("sp", lambda e: e.dma_start(out=ngb[:], in_=bcast_rows(I["hyena_norm_g"], 128)), writes=["ngb"], dma=True)
        zin_keys = ["zin"] + [f"zin{t}" for t in range(NT)]
        for n in range(2):
            def cload(cc2):
                mb = cc2 % 2
                S.op("sp", lambda e: e.dma_start(out=mtc[mb][:], in_=C.MDc[cc2]), writes=[f"cmtc{mb}"], dma=True)
                S.op("sp", lambda e: e.dma_start(out=mts[mb][:], in_=C.MDs[cc2]), writes=[f"cmts{mb}"], dma=True)
            if n == 0:
                cload(0)
            for cc2 in range(16):
                mb = cc2 % 2
                cload((cc2 + 1) % 16)
                for sub in range(2):
                    fc = cc2 * 2 + sub
                    gb = fc % 2
                    S.op("sp", lambda e, n=n, fc=fc, gb=gb: e.dma_start(out=gre[gb][:], in_=C.GD[n, 0, fc]),
                         writes=[f"cgre{gb}"], dma=True)
                    S.op("sp", lambda e, n=n, fc=fc, gb=gb: e.dma_start(out=gim[gb][:], in_=C.GD[n, 1, fc]),
                         writes=[f"cgim{gb}"], dma=True)
                    pc, ps_ = (0, 1) if gb == 0 else (2, 3)
                    for (pb, mt, mk) in ((pc, mtc, "cmtc"), (ps_, mts, "cmts")):
                        for rc in range(NT):
                            S.op("pe", lambda e, pb=pb, mt=mt, mb=mb, rc=rc, sub=sub: e.matmul(
                                C.ps[pb][:], lhsT=mt[mb][:, rc, sub * 128:(sub + 1) * 128], rhs=zin[:, rc, :],
                                start=(rc == 0), stop=(rc == NT - 1)),
                                reads=[f"{mk}{mb}"] + zin_keys, writes=[f"ps{pb}"])
                    S.op("act", lambda e, gb=gb, pc=pc: e.copy(out=xc[gb][:], in_=C.ps[pc][:]),
                         reads=[f"ps{pc}"], writes=["xc0"])
                    S.op("act", lambda e, gb=gb, ps_=ps_: e.copy(out=xs[gb][:], in_=C.ps[ps_][:]),
                         reads=[f"ps{ps_}"], writes=["xs0"])
                    S.op("dve", lambda e, gb=gb: e.tensor_tensor(out=ta[:], in0=xc[gb][:], in1=gre[gb][:], op=ALU.mult),
                         reads=["xc0", f"cgre{gb}"], writes=["cta"])
                    S.op("dve", lambda e, gb=gb: e.tensor_tensor(out=tb[:], in0=xs[gb][:], in1=gim[gb][:], op=ALU.mult),
                         reads=["xs0", f"cgim{gb}"], writes=["ctb"])
                    S.op("dve", lambda e, fc=fc: e.tensor_tensor(out=wre[:, fc, :], in0=ta[:], in1=tb[:], op=ALU.add),
                         reads=["cta", "ctb"], writes=[f"wre{fc}"])
                    S.op("pool", lambda e, gb=gb: e.tensor_tensor(out=tc_[:], in0=xs[gb][:], in1=gre[gb][:], op=ALU.mult),
                         reads=["xs0", f"cgre{gb}"], writes=["ctc"])
                    S.op("pool", lambda e, gb=gb: e.tensor_tensor(out=td[:], in0=xc[gb][:], in1=gim[gb][:], op=ALU.mult),
                         reads=["xc0", f"cgim{gb}"], writes=["ctd"])
                    S.op("pool", lambda e, fc=fc: e.tensor_tensor(out=wim[:, fc, :], in0=tc_[:], in1=td[:],
                                                                  op=ALU.subtract),
                         reads=["ctc", "ctd"], writes=[f"wim{fc}"])
            wre_keys = [f"wre{f}" for f in range(NT)]
            wim_keys = [f"wim{f}" for f in range(NT)]
            for cc2 in range(16):
                mb = cc2 % 2
                if not (n == 1 and cc2 == 15):
                    cload((cc2 + 1) % 16)
                for sub in range(2):
                    tcx = cc2 * 2 + sub
                    gb = tcx % 2
                    gsrc = C.U[n]
                    zsrc = C.U[2] if n == 0 else C.Z1
                    S.op("sp", lambda e, gb=gb, gsrc=gsrc, tcx=tcx: e.dma_start(
                        out=gate[gb][:], in_=gsrc[tcx * 128:(tcx + 1) * 128, :]), writes=[f"gate{gb}"], dma=True)
                    S.op("sp", lambda e, gb=gb, zsrc=zsrc, tcx=tcx: e.dma_start(
                        out=zf[gb][:], in_=zsrc[tcx * 128:(tcx + 1) * 128, :]),
                        reads=([f"Z1_{tcx}"] if n == 1 else []), writes=[f"zf{gb}"], dma=True)
                    pb = 4 + gb
                    for fcx in range(NT):
                        S.op("pe", lambda e, pb=pb, mb=mb, fcx=fcx, sub=sub: e.matmul(
                            C.ps[pb][:], lhsT=mtc[mb][:, fcx, sub * 128:(sub + 1) * 128], rhs=wre[:, fcx, :],
                            start=(fcx == 0), stop=False),
                            reads=[f"cmtc{mb}", wre_keys[fcx]], writes=[f"ps{pb}"])
                        S.op("pe", lambda e, pb=pb, mb=mb, fcx=fcx, sub=sub: e.matmul(
                            C.ps[pb][:], lhsT=mts[mb][:, fcx, sub * 128:(sub + 1) * 128], rhs=wim[:, fcx, :],
                            start=False, stop=(fcx == NT - 1)),
                            reads=[f"cmts{mb}", wim_keys[fcx]], writes=[f"ps{pb}"])
                    S.op("dve", lambda e, gb=gb, n=n: e.tensor_tensor(out=zo[gb][:], in0=zf[gb][:], in1=skipb[:, n, :],
                                                                     op=ALU.mult),
                         reads=[f"zf{gb}", f"skipb{n}"], writes=[f"zo{gb}"])
                    S.op("dve", lambda e, gb=gb, pb=pb: e.tensor_tensor(out=zo[gb][:], in0=zo[gb][:], in1=C.ps[pb][:],
                                                                       op=ALU.add),
                         reads=[f"zo{gb}", f"ps{pb}"], writes=[f"zo{gb}"])
                    S.op("dve", lambda e, gb=gb: e.tensor_tensor(out=zo[gb][:], in0=zo[gb][:], in1=gate[gb][:],
                                                                op=ALU.mult),
                         reads=[f"zo{gb}", f"gate{gb}"], writes=[f"zo{gb}"])
                    if n == 0:
                        S.op("act", lambda e, gb=gb, tcx=tcx: e.dma_start(out=C.Z1[tcx * 128:(tcx + 1) * 128, :],
                                                                          in_=zo[gb][:]),
                             reads=[f"zo{gb}"], writes=[f"Z1_{tcx}"], dma=True)
                        S.op("act", lambda e, gb=gb, tcx=tcx: e.copy(out=zin[:, tcx, :], in_=zo[gb][:]),
                             reads=[f"zo{gb}"], writes=[f"zin{tcx}"])
                    else:
                        S.op("pool", lambda e, gb=gb: e.tensor_tensor(out=sq[:], in0=zo[gb][:], in1=zo[gb][:], op=ALU.mult),
                             reads=[f"zo{gb}"], writes=["cta"])
                        S.op("dve", lambda e: e.reduce_sum(out=gs[:], in_=sq[:].rearrange("p (g c) -> p g c", g=8),
                                                           axis=AX.X),
                             reads=["cta"], writes=["cgs"])
                        S.op("act", lambda e: e.activation(out=gs[:], in_=gs[:], func=AF.Sqrt, bias=float(EPS),
                                                           scale=1.0 / 64.0), reads=["cgs"], writes=["cgs"])
                        S.op("dve", lambda e: e.reciprocal(gs[:], gs[:]), reads=["cgs"], writes=["cgs"])
                        S.op("dve", lambda e, gb=gb: e.tensor_tensor(
                            out=zn[:].rearrange("p (g c) -> p g c", g=8),
                            in0=zo[gb][:].rearrange("p (g c) -> p g c", g=8),
                            in1=gs[:].unsqueeze(2).to_broadcast([128, 8, 64]), op=ALU.mult),
                            reads=[f"zo{gb}", "cgs"], writes=["ctb"])
                        S.op("pool", lambda e: e.tensor_tensor(out=zn[:], in0=zn[:], in1=ngb[:], op=ALU.mult),
                             reads=["ctb", "ngb"], writes=["ctb"])
                        pt = 6 + gb
                        for k in range(4):
                            S.op("pe", lambda e, pt=pt, k=k: e.transpose(
                                out=C.ps[pt][:, k * 128:(k + 1) * 128], in_=zn[:, k * 128:(k + 1) * 128],
                                identity=C.ident[:]), reads=["ctb", "ident"], writes=[f"ps{pt}"])
                        S.op("act", lambda e, pt=pt, gb=gb: e.copy(out=mxt[gb][:],
                                                                   in_=C.ps[pt][:].rearrange("p (k t) -> p k t", k=4)),
                             reads=[f"ps{pt}"], writes=[f"mxt{gb}"])
                        S.op("act", lambda e, gb=gb, tcx=tcx: e.dma_start(
                            out=C.MIXT[0:4, :, tcx * 128:(tcx + 1) * 128].rearrange("k p t -> p k t"), in_=mxt[gb][:]),
                            reads=[f"mxt{gb}"], writes=[f"MIXTh_{tcx}"], dma=True)
        S.flush()


def phase_oproj(C):
    nc, S, I = C.nc, C.S, C.I
    C.X2 = C.dscratch("X2", [L, D], F32)
    C.H2 = C.dscratch("H2", [L, D], BF16)
    C.aff = C.stack.enter_context(nc.sbuf_tensor("aff", [128, NT, NE], F32))
    with ExitStack() as st:
        def sb(name, shape, dt):
            return st.enter_context(nc.sbuf_tensor(name, shape, dt))

        mix = sb("mix", [128, 8, L], BF16)
        wst = sb("owst", [128, 8, 512], F32)
        wbf = sb("owbf", [128, 8, D], BF16)
        g32 = sb("og32", [128, D], F32)
        wr = sb("owr", [128, 8, NE], F32)
        xt = [sb(f"oxt{i}", [128, D], F32) for i in range(2)]
        x2 = [sb(f"ox2{i}", [128, D], F32) for i in range(2)]
        h2 = [sb(f"oh2{i}", [128, D], F32) for i in range(2)]
        h2b = [sb(f"oh2b{i}", [128, D], BF16) for i in range(2)]
        h2T = sb("oh2T", [128, 8, 128], F32)
        junk = sb("ojunk", [128, D], F32)
        ss = [sb(f"oss{i}", [128, 1], F32) for i in range(2)]
        rstd = [sb(f"orstd{i}", [128, 1], F32) for i in range(2)]
        lg = sb("olg", [128, NE], F32)
        mx = sb("omx", [128, 1], F32)
        es = sb("oes", [128, 1], F32)

        for m in range(8):
            S.op("sp", lambda e, m=m: e.dma_start(out=mix[:, m, :], in_=C.MIXT[m]), writes=[f"mix{m}"], dma=True)
        mix_keys = [f"mix{m}" for m in range(8)]
        wv = I["w_out"].rearrange("(k p) n -> p k n", p=128)
        for half in range(2):
            S.op("sp", lambda e, half=half: e.dma_start(out=wst[:], in_=wv[:, :, half * 512:(half + 1) * 512]),
                 writes=["owst"], dma=True)
            S.op("act", lambda e, half=half: e.copy(out=wbf[:, :, half * 512:(half + 1) * 512], in_=wst[:]),
                 reads=["owst"], writes=[f"owbf{half}"])
        S.op("sp", lambda e: e.dma_start(out=g32[:], in_=bcast_rows(I["ffn_norm_g"], 128)), writes=["og32"], dma=True)
        S.op("dve", lambda e: e.tensor_scalar(g32[:], g32[:], 32.0, None, ALU.mult), reads=["og32"], writes=["og32"])
        S.op("sp", lambda e: e.dma_start(out=wr[:], in_=I["w_router"].rearrange("(k p) n -> p k n", p=128)),
             writes=["owr"], dma=True)
        for i in range(NT):
            b = i % 2
            S.op("sp", lambda e, i=i, b=b: e.dma_start(out=xt[b][:], in_=I["x"][i * 128:(i + 1) * 128, :]),
                 writes=[f"oxt{b}"], dma=True)
            for half in range(2):
                pb = half
                for m in range(8):
                    S.op("pe", lambda e, pb=pb, m=m, i=i, half=half: e.matmul(
                        C.ps[pb][:], lhsT=mix[:, m, i * 128:(i + 1) * 128], rhs=wbf[:, m, half * 512:(half + 1) * 512],
                        start=(m == 0), stop=(m == 7)), reads=[mix_keys[m], f"owbf{half}"], writes=[f"ps{pb}"])
                S.op("dve", lambda e, pb=pb, b=b, half=half: e.tensor_tensor(
                    out=x2[b][:, half * 512:(half + 1) * 512], in0=C.ps[pb][:], in1=xt[b][:, half * 512:(half + 1) * 512],
                    op=ALU.add), reads=[f"ps{pb}", f"oxt{b}"], writes=[f"ox2{b}_{half}"])
            x2k = [f"ox2{b}_0", f"ox2{b}_1"]
            S.op("pool", lambda e, i=i, b=b: e.dma_start(out=C.X2[i * 128:(i + 1) * 128, :], in_=x2[b][:]),
                 reads=x2k, writes=[f"X2_{i}"], dma=True)
            S.op("dve", lambda e, b=b: e.memset(ss[b][:], 0.0), writes=[f"oss{b}"])
            S.op("act", lambda e, b=b: e.activation(out=junk[:], in_=x2[b][:], func=AF.Square, accum_out=ss[b][:]),
                 reads=x2k + [f"oss{b}"], writes=[f"oss{b}", "ojunk"])
            S.op("act", lambda e, b=b: e.activation(out=ss[b][:], in_=ss[b][:], func=AF.Sqrt, bias=float(D * EPS),
                                                    scale=1.0), reads=[f"oss{b}"], writes=[f"oss{b}"])
            S.op("dve", lambda e, b=b: e.reciprocal(rstd[b][:], ss[b][:]), reads=[f"oss{b}"], writes=[f"orstd{b}"])
            S.op("dve", lambda e, b=b: e.scalar_tensor_tensor(out=h2[b][:], in0=x2[b][:], scalar=rstd[b][:, 0:1],
                                                              in1=g32[:], op0=ALU.mult, op1=ALU.mult),
                 reads=x2k + [f"orstd{b}", "og32"], writes=[f"oh2{b}"])
            S.op("act", lambda e, b=b: e.copy(out=h2b[b][:], in_=h2[b][:]), reads=[f"oh2{b}"], writes=[f"oh2b{b}"])
            S.op("pool", lambda e, i=i, b=b: e.dma_start(out=C.H2[i * 128:(i + 1) * 128, :], in_=h2b[b][:]),
                 reads=[f"oh2b{b}"], writes=[f"H2_{i}"], dma=True)
            for half in range(2):
                pb = 2 + half
                for kk in range(4):
                    k = half * 4 + kk
                    S.op("pe", lambda e, b=b, k=k, kk=kk, pb=pb: e.transpose(
                        out=C.ps[pb][:, kk * 128:(kk + 1) * 128], in_=h2[b][:, k * 128:(k + 1) * 128],
                        identity=C.ident[:]), reads=[f"oh2{b}", "ident"], writes=[f"ps{pb}"])
                eng = "act" if half == 0 else "dve"
                if eng == "act":
                    S.op("act", lambda e, half=half, pb=pb: e.copy(
                        out=h2T[:, half * 4:(half + 1) * 4, :], in_=C.ps[pb][:].rearrange("p (k t) -> p k t", k=4)),
                        reads=[f"ps{pb}"], writes=[f"oh2T{half}"])
                else:
                    S.op("dve", lambda e, half=half, pb=pb: e.tensor_copy(
                        out=h2T[:, half * 4:(half + 1) * 4, :], in_=C.ps[pb][:].rearrange("p (k t) -> p k t", k=4)),
                        reads=[f"ps{pb}"], writes=[f"oh2T{half}"])
            pl = 4 + b
            for k in range(8):
                S.op("pe", lambda e, pl=pl, k=k: e.matmul(C.ps[pl][:, 0:NE], lhsT=h2T[:, k, :], rhs=wr[:, k, :],
                                                          start=(k == 0), stop=(k == 7)),
                     reads=[f"oh2T{k // 4}", "owr"], writes=[f"ps{pl}"])
            S.op("dve", lambda e, pl=pl: e.tensor_copy(lg[:], C.ps[pl][:, 0:NE]), reads=[f"ps{pl}"], writes=["olg"])
            S.op("dve", lambda e: e.reduce_max(out=mx[:], in_=lg[:], axis=AX.X), reads=["olg"], writes=["omx"])
            S.op("dve", lambda e: e.tensor_scalar(mx[:], mx[:], -1.0, None, ALU.mult), reads=["omx"], writes=["omx"])
            S.op("dve", lambda e: e.memset(es[:], 0.0), writes=["oes"])
            S.op("act", lambda e: e.activation(out=lg[:], in_=lg[:], func=AF.Exp, bias=mx[:, 0:1], scale=1.0,
                                               accum_out=es[:]), reads=["olg", "omx", "oes"], writes=["olg", "oes"])
            S.op("dve", lambda e: e.reciprocal(es[:], es[:]), reads=["oes"], writes=["oes"])
            S.op("dve", lambda e, i=i: e.tensor_scalar(C.aff[:, i, :], lg[:], es[:, 0:1], None, ALU.mult),
                 reads=["olg", "oes"], writes=[f"aff{i}"])
        S.flush()


def phase_topk(C):
    nc, S, I = C.nc, C.S, C.I
    C.idx_i = C.stack.enter_context(nc.sbuf_tensor("idx_i", [128, NE * 4], I32))
    C.gsel = C.stack.enter_context(nc.sbuf_tensor("gsel", [128, NE * 4], F32))
    with ExitStack() as st:
        def sb(name, shape, dt):
            return st.enter_context(nc.sbuf_tensor(name, shape, dt))

        maskT = sb("tmaskT", [128, NT, NE], F32)
        pre = sb("tpre", [128, NT, NE], F32)
        slot = sb("tslot", [128, NT, NE], F32)
        tri = sb("ttri", [128, 128], F32)
        jio = sb("tjio", [128, CAP], F32)
        tg = sb("ttg", [128, NT, NE, 5], BF16)
        tokp = sb("ttokp", [128, NT], F32)
        toki = sb("ttoki", [128, NT], F32)
        a1 = sb("ta1", [128, NT, NE], BF16)
        a2 = sb("ta2", [128, NT, NE], BF16)
        a3 = sb("ta3", [128, NT, NE], BF16)
        rr = sb("trr", [128, NT, NE], F32)
        af32 = sb("taf32", [128, NT, NE], F32)
        ssel = [sb(f"tssel{i}", [128, CAP], BF16) for i in range(4)]
        ig = sb("tig", [128, 5], F32)
        idf = sb("tidf", [128, 1], F32)

        thr = sb("tthr", [128, NE], F32)
        cmpt = sb("tcmp", [128, NT, NE], F32)
        cnt = sb("tcnt", [128, NE], F32)
        ge = sb("tge", [128, NE], F32)
        S.op("dve", lambda e: e.memset(thr[:], 0.5), writes=["tthr"])
        NIT = 24
        for it in range(NIT):
            pb = it % 2
            step = 0.5 ** (it + 1)
            S.op("dve", lambda e: e.tensor_tensor(out=cmpt[:], in0=C.aff[:],
                                                  in1=thr[:].unsqueeze(1).to_broadcast([128, NT, NE]), op=ALU.is_gt),
                 reads=["tthr"], writes=["tcmp"])
            S.op("dve", lambda e: e.reduce_sum(out=cnt[:], in_=cmpt[:].rearrange("p i e -> p e i"), axis=AX.X),
                 reads=["tcmp"], writes=["tcnt"])
            S.op("pe", lambda e, pb=pb: e.matmul(C.ps[pb][:, 0:NE], lhsT=C.ones_f[:], rhs=cnt[:], start=True, stop=True),
                 reads=["ones_f", "tcnt"], writes=[f"ps{pb}"])
            S.op("dve", lambda e, pb=pb: e.tensor_scalar(ge[:], C.ps[pb][:, 0:NE], float(CAP) - 0.5, -0.5, ALU.is_gt, ALU.add),
                 reads=[f"ps{pb}"], writes=["tge"])
            S.op("dve", lambda e, step=step: e.scalar_tensor_tensor(out=thr[:], in0=ge[:], scalar=float(step), in1=thr[:],
                                                                    op0=ALU.mult, op1=ALU.add),
                 reads=["tge", "tthr"], writes=["tthr"])
        S.op("dve", lambda e: e.tensor_scalar(thr[:], thr[:], float(-(0.5 ** (NIT + 1))), None, ALU.add),
             reads=["tthr"], writes=["tthr"])
        S.op("dve", lambda e: e.tensor_tensor(out=maskT[:], in0=C.aff[:],
                                              in1=thr[:].unsqueeze(1).to_broadcast([128, NT, NE]), op=ALU.is_gt),
             reads=["tthr"], writes=[f"tmaskT{i}" for i in range(NT)])
        S.op("pool", lambda e: e.memset(pre[:, 0, :], 0.0), writes=["tpre0"])
        for i in range(1, NT):
            S.op("pool", lambda e, i=i: e.tensor_tensor(out=pre[:, i, :], in0=pre[:, i - 1, :], in1=maskT[:, i - 1, :],
                                                        op=ALU.add),
                 reads=[f"tpre{i - 1}", f"tmaskT{i - 1}"], writes=[f"tpre{i}"])
        S.op("pool", lambda e: e.iota(tri[:], [[1, 128]], base=0, channel_multiplier=-1,
                                      allow_small_or_imprecise_dtypes=True), writes=["ttri"])
        S.op("dve", lambda e: e.tensor_scalar(tri[:], tri[:], 0.0, None, ALU.is_gt), reads=["ttri"], writes=["ttri"])
        for i in range(NT):
            pb = 2 + i % 2
            S.op("pe", lambda e, i=i, pb=pb: e.matmul(C.ps[pb][:, 0:NE], lhsT=tri[:], rhs=maskT[:, i, :],
                                                      start=True, stop=False),
                 reads=["ttri", f"tmaskT{i}"], writes=[f"ps{pb}"])
            S.op("pe", lambda e, i=i, pb=pb: e.matmul(C.ps[pb][:, 0:NE], lhsT=C.ones_f[:], rhs=pre[:, i, :],
                                                      start=False, stop=True),
                 reads=["ones_f", f"tpre{i}"], writes=[f"ps{pb}"])
            S.op("dve", lambda e, i=i, pb=pb: e.scalar_tensor_tensor(out=slot[:, i, :], in0=C.ps[pb][:, 0:NE], scalar=1.0,
                                                                     in1=maskT[:, i, :], op0=ALU.add, op1=ALU.mult),
                 reads=[f"ps{pb}", f"tmaskT{i}"], writes=[f"tslot{i}"])
            S.op("dve", lambda e, i=i: e.tensor_scalar(slot[:, i, :], slot[:, i, :], -1.0, None, ALU.add),
                 reads=[f"tslot{i}"], writes=[f"tslot{i}"])
        S.op("pool", lambda e: e.iota(jio[:], [[1, CAP]], base=0, channel_multiplier=0,
                                      allow_small_or_imprecise_dtypes=True), writes=["tjio"])
        S.op("pool", lambda e: e.iota(toki[:], [[1, NT]], base=0, channel_multiplier=0,
                                      allow_small_or_imprecise_dtypes=True), writes=["ttoki"])
        S.op("pool", lambda e: e.iota(tokp[:], [[0, NT]], base=0, channel_multiplier=1,
                                      allow_small_or_imprecise_dtypes=True), writes=["ttokp"])
        aff_keys = []
        S.op("dve", lambda e: e.tensor_copy(tg[:, :, :, 0], toki[:].unsqueeze(2).to_broadcast([128, NT, NE])),
             reads=["ttoki"], writes=["ttg0"])
        S.op("dve", lambda e: e.tensor_copy(tg[:, :, :, 1], tokp[:].unsqueeze(2).to_broadcast([128, NT, NE])),
             reads=["ttokp"], writes=["ttg1"])
        S.op("dve", lambda e: e.tensor_copy(a1[:], C.aff[:]), writes=["ta1"])
        S.op("dve", lambda e: e.tensor_copy(af32[:], a1[:]), reads=["ta1"], writes=["taf32"])
        S.op("dve", lambda e: e.tensor_tensor(out=rr[:], in0=C.aff[:], in1=af32[:], op=ALU.subtract),
             reads=["taf32"], writes=["trr"])
        S.op("dve", lambda e: e.tensor_copy(a2[:], rr[:]), reads=["trr"], writes=["ta2"])
        S.op("dve", lambda e: e.tensor_copy(af32[:], a2[:]), reads=["ta2"], writes=["taf32"])
        S.op("dve", lambda e: e.tensor_tensor(out=rr[:], in0=rr[:], in1=af32[:], op=ALU.subtract),
             reads=["taf32", "trr"], writes=["trr"])
        S.op("dve", lambda e: e.tensor_copy(a3[:], rr[:]), reads=["trr"], writes=["ta3"])
        for c_, (a_, k_) in enumerate(((a1, "ta1"), (a2, "ta2"), (a3, "ta3"))):
            S.op("pool", lambda e, c_=c_, a_=a_: e.tensor_copy(tg[:, :, :, 2 + c_], a_[:]),
                 reads=[k_], writes=[f"ttg{2 + c_}"])
        tgk = [f"ttg{c_}" for c_ in range(5)]
        sc = 0
        for ex in range(NE):
            base = 0 if ex % 2 == 0 else 4
            for i in range(NT):
                sbk = sc % 4
                sc += 1
                S.op("dve", lambda e, i=i, ex=ex, sbk=sbk: e.tensor_scalar(ssel[sbk][:], jio[:], slot[:, i, ex:ex + 1], None,
                                                                          ALU.is_equal),
                     reads=["tjio", f"tslot{i}"], writes=[f"tssel{sbk}"])
                for jc in range(4):
                    S.op("pe", lambda e, i=i, ex=ex, sbk=sbk, jc=jc, base=base: e.matmul(
                        C.ps[base + jc][:, 0:5], lhsT=ssel[sbk][:, jc * 128:(jc + 1) * 128], rhs=tg[:, i, ex, :],
                        start=(i == 0), stop=(i == NT - 1)),
                        reads=[f"tssel{sbk}"] + tgk, writes=[f"ps{base + jc}"])
            for jc in range(4):
                col = ex * 4 + jc
                S.op("act", lambda e, jc=jc, base=base: e.copy(out=ig[:], in_=C.ps[base + jc][:, 0:5]),
                     reads=[f"ps{base + jc}"], writes=["tig"])
                S.op("dve", lambda e: e.scalar_tensor_tensor(out=idf[:], in0=ig[:, 0:1], scalar=128.0, in1=ig[:, 1:2],
                                                             op0=ALU.mult, op1=ALU.add),
                     reads=["tig"], writes=["tidf"])
                S.op("dve", lambda e, col=col: e.tensor_copy(C.idx_i[:, col:col + 1], idf[:]),
                     reads=["tidf"], writes=[f"idx{col}"])
                S.op("dve", lambda e, col=col: e.tensor_tensor(out=C.gsel[:, col:col + 1], in0=ig[:, 2:3], in1=ig[:, 3:4],
                                                               op=ALU.add),
                     reads=["tig"], writes=[f"gsel{col}"])
                S.op("dve", lambda e, col=col: e.tensor_tensor(out=C.gsel[:, col:col + 1], in0=C.gsel[:, col:col + 1],
                                                               in1=ig[:, 4:5], op=ALU.add),
                     reads=["tig", f"gsel{col}"], writes=[f"gsel{col}"])
        S.flush()


def phase_moe(C):
    nc, S, I = C.nc, C.S, C.I
    with ExitStack() as st:
        def sb(name, shape, dt):
            return st.enter_context(nc.sbuf_tensor(name, shape, dt))

        xg = [sb(f"xg{j}", [128, D], BF16) for j in range(4)]
        xgT = sb("xgT", [128, 8, CAP], BF16)
        hT = sb("hT", [128, NFC, CAP], BF16)
        NWB = 4
        PF = NWB - 1
        wgt = [sb(f"wgt{i}", [128, 8, 256], BF16) for i in range(NWB)]
        wut = [sb(f"wut{i}", [128, 8, 256], BF16) for i in range(NWB)]
        wdt = [sb(f"wdt{i}", [128, NFC, D], BF16) for i in range(2)]
        sa = [sb(f"sa{i}", [128, CAP], F32) for i in range(2)]
        yy = [sb(f"yy{i}", [128, D], F32) for i in range(4)]
        psb = [C.ps[6][:].bitcast(BF16), C.ps[7][:].bitcast(BF16)]
        NFB = NFC // 2
        blocks = [(ex, fb) for ex in range(NE) for fb in range(NFB)]

        NPRE = getattr(C, "NPRE", 0)

        def emit_wload(bi):
            ex, fb = blocks[bi]
            wb = bi % NWB
            if ex < NPRE:
                q_, wg_src, wu_src = "sp", C.WGB[ex], C.WUB[ex]
            else:
                q_, wg_src, wu_src = "pool", I["w_gate"][ex], I["w_up"][ex]
            wgv = wg_src.rearrange("(k p) f -> p k f", p=128)
            wuv = wu_src.rearrange("(k p) f -> p k f", p=128)
            S.op(q_, lambda e: e.dma_start(out=wgt[wb][:], in_=wgv[:, :, fb * 256:(fb + 1) * 256]),
                 writes=[f"wgt{wb}"], dma=True)
            S.op(q_, lambda e: e.dma_start(out=wut[wb][:], in_=wuv[:, :, fb * 256:(fb + 1) * 256]),
                 writes=[f"wut{wb}"], dma=True)

        def emit_wd(ex):
            wdb = ex % 2
            if ex < NPRE:
                q_, src = "sp", C.WDB[ex]
            else:
                q_, src = "pool", I["w_down"][ex]
            wdv = src.rearrange("(c p) d -> p c d", p=128)
            for hh in range(2):
                S.op(q_, lambda e, hh=hh: e.dma_start(
                    out=wdt[wdb][:, hh * 11:(hh + 1) * 11, :], in_=wdv[:, hh * 11:(hh + 1) * 11, :]),
                    writes=[f"wdt{wdb}_{hh}"], dma=True)

        def emit_gather(ex):
            for jc in range(4):
                col = ex * 4 + jc
                S.op("pool", lambda e, jc=jc, col=col: e.indirect_dma_start(
                    out=xg[jc][:], out_offset=None, in_=C.H2,
                    in_offset=bass.IndirectOffsetOnAxis(ap=C.idx_i[:, col:col + 1], axis=0)),
                    writes=[f"xg{jc}"], dma=True)

        pending_sc = []

        def flush_scatters(n):
            for _ in range(min(n, len(pending_sc))):
                yb, col = pending_sc.pop(0)
                S.op("pool", lambda e, yb=yb, col=col: e.indirect_dma_start(
                    out=C.X2, out_offset=bass.IndirectOffsetOnAxis(ap=C.idx_i[:, col:col + 1], axis=0),
                    in_=yy[yb][:], in_offset=None, compute_op=ALU.add, oob_is_err=True),
                    reads=[f"yy{yb}_0", f"yy{yb}_1"], writes=["X2"], dma=True)

        emit_gather(0)
        for bi in range(PF):
            emit_wload(bi)
        emit_wd(0)
        gu = 0
        yc = 0
        bi = 0
        for ex in range(NE):
            for jc in range(4):
                pb = jc % 2
                for k in range(8):
                    S.op("pe", lambda e, jc=jc, k=k, pb=pb: e.transpose(
                        out=psb[pb][:, k * 128:(k + 1) * 128], in_=xg[jc][:, k * 128:(k + 1) * 128],
                        identity=C.identb[:]), reads=[f"xg{jc}", "identb"], writes=[f"ps{6 + pb}"])
                if jc % 2 == 0:
                    S.op("act", lambda e, jc=jc, pb=pb: e.copy(out=xgT[:, :, jc * 128:(jc + 1) * 128],
                                                               in_=psb[pb][:].rearrange("p (k t) -> p k t", k=8)),
                         reads=[f"ps{6 + pb}"], writes=[f"xgT{jc}"])
                else:
                    S.op("dve", lambda e, jc=jc, pb=pb: e.tensor_copy(out=xgT[:, :, jc * 128:(jc + 1) * 128],
                                                                      in_=psb[pb][:].rearrange("p (k t) -> p k t", k=8)),
                         reads=[f"ps{6 + pb}"], writes=[f"xgT{jc}"])
            if ex + 1 < NE:
                emit_gather(ex + 1)
                emit_wd(ex + 1)
            xk = [f"xgT{jc}" for jc in range(4)]
            wdb = ex % 2
            for fb in range(NFB):
                if bi + PF < len(blocks):
                    emit_wload(bi + PF)
                if fb in (1, 2, 3, 4):
                    flush_scatters(1)
                wb = bi % NWB
                bi += 1
                for sub in range(2):
                    fc = fb * 2 + sub
                    g2 = gu % 2
                    gu += 1
                    pa, pu = (0, 1) if g2 == 0 else (2, 3)
                    for (pb, wt, wk) in ((pa, wgt, "wgt"), (pu, wut, "wut")):
                        for k in range(8):
                            S.op("pe", lambda e, pb=pb, wt=wt, wb=wb, k=k, sub=sub: e.matmul(
                                C.ps[pb][:], lhsT=wt[wb][:, k, sub * 128:(sub + 1) * 128], rhs=xgT[:, k, :],
                                start=(k == 0), stop=(k == 7)), reads=[f"{wk}{wb}"] + xk, writes=[f"ps{pb}"])
                    S.op("act", lambda e, g2=g2, pa=pa: e.activation(out=sa[g2][:], in_=C.ps[pa][:], func=AF.Silu),
                         reads=[f"ps{pa}"], writes=[f"sa{g2}"])
                    S.op("dve", lambda e, g2=g2, pu=pu, fc=fc: e.tensor_tensor(out=hT[:, fc, :], in0=sa[g2][:],
                                                                              in1=C.ps[pu][:], op=ALU.mult),
                         reads=[f"sa{g2}", f"ps{pu}"], writes=[f"hT{fc}"])
            hk = [f"hT{fc}" for fc in range(NFC)]
            for jc in range(4):
                col = ex * 4 + jc
                yb = yc % 4
                yc += 1
                for dh in range(2):
                    pb = 4 + dh
                    for fc in range(NFC):
                        S.op("pe", lambda e, pb=pb, fc=fc, jc=jc, dh=dh, wdb=wdb: e.matmul(
                            C.ps[pb][:], lhsT=hT[:, fc, jc * 128:(jc + 1) * 128], rhs=wdt[wdb][:, fc, dh * 512:(dh + 1) * 512],
                            start=(fc == 0), stop=(fc == NFC - 1)),
                            reads=[hk[fc], f"wdt{wdb}_{fc // 11}"], writes=[f"ps{pb}"])
                    if dh == 0:
                        S.op("act", lambda e, pb=pb, yb=yb, col=col: e.mul(
                            out=yy[yb][:, 0:512], in_=C.ps[pb][:], mul=C.gsel[:, col:col + 1]),
                            reads=[f"ps{pb}"], writes=[f"yy{yb}_0"])
                    else:
                        S.op("dve", lambda e, pb=pb, yb=yb, col=col: e.tensor_scalar(
                            yy[yb][:, 512:1024], C.ps[pb][:], C.gsel[:, col:col + 1], None, ALU.mult),
                            reads=[f"ps{pb}"], writes=[f"yy{yb}_1"])
                pending_sc.append((yb, col))
            if ex == NE - 1:
                flush_scatters(len(pending_sc))
        S.flush()


def phase_final(C):
    nc, S, I = C.nc, C.S, C.I
    with ExitStack() as st:
        def sb(name, shape, dt):
            return st.enter_context(nc.sbuf_tensor(name, shape, dt))

        g32 = sb("fg32", [128, D], F32)
        xt = [sb(f"fxt{i}", [128, D], F32) for i in range(3)]
        ot = [sb(f"fot{i}", [128, D], F32) for i in range(3)]
        junk = sb("fjunk", [128, D], F32)
        ss = [sb(f"fss{i}", [128, 1], F32) for i in range(3)]
        rstd = [sb(f"frstd{i}", [128, 1], F32) for i in range(3)]
        S.op("sp", lambda e: e.dma_start(out=g32[:], in_=bcast_rows(I["final_norm_g"], 128)), writes=["fg32"], dma=True)
        S.op("dve", lambda e: e.tensor_scalar(g32[:], g32[:], 32.0, None, ALU.mult), reads=["fg32"], writes=["fg32"])
        for i in range(NT):
            b = i % 3
            S.op("sp", lambda e, i=i, b=b: e.dma_start(out=xt[b][:], in_=C.X2[i * 128:(i + 1) * 128, :]),
                 writes=[f"fxt{b}"], dma=True)
            S.op("dve", lambda e, b=b: e.memset(ss[b][:], 0.0), writes=[f"fss{b}"])
            S.op("act", lambda e, b=b: e.activation(out=junk[:], in_=xt[b][:], func=AF.Square, accum_out=ss[b][:]),
                 reads=[f"fxt{b}", f"fss{b}"], writes=[f"fss{b}", "fjunk"])
            S.op("act", lambda e, b=b: e.activation(out=ss[b][:], in_=ss[b][:], func=AF.Sqrt, bias=float(D * EPS),
                                                    scale=1.0), reads=[f"fss{b}"], writes=[f"fss{b}"])
            S.op("dve", lambda e, b=b: e.reciprocal(rstd[b][:], ss[b][:]), reads=[f"fss{b}"], writes=[f"frstd{b}"])
            S.op("dve", lambda e, b=b: e.scalar_tensor_tensor(out=ot[b][:], in0=xt[b][:], scalar=rstd[b][:, 0:1],
                                                              in1=g32[:], op0=ALU.mult, op1=ALU.mult),
                 reads=[f"fxt{b}", f"frstd{b}", "fg32"], writes=[f"fot{b}"])
            S.op("pool", lambda e, i=i, b=b: e.dma_start(out=C.out[i * 128:(i + 1) * 128, :], in_=ot[b][:]),
                 reads=[f"fot{b}"], writes=[f"out{i}"], dma=True)
        S.flush()
```
